# Optimizing a Trainium2 kernel written in Bass

```python
import math
import jax, jax.numpy as jnp
from jax import lax
import numpy as np

D_MODEL = 1024
BATCH = 32
SEQ = 2048
DEPTH = 1

N_MEM = 256
RMS_EPS = 1e-6
POOL_WINDOWS = (2, 4, 8, 16)
POOL_WIDTH = D_MODEL // 4
POOL_GROUP = POOL_WIDTH // len(POOL_WINDOWS)
DA_HEADS = 4
DA_HEAD_DIM = 64
DA_V_DIM = 2 * DA_HEAD_DIM
DA_QK_WIDTH = DA_HEADS * 2 * DA_HEAD_DIM
DA_WIDTH = DA_HEADS * DA_V_DIM
Q_BLOCK = 128
MEM_HEADS = 4
MEM_HEAD_DIM = 64
MEM_WIDTH = MEM_HEADS * MEM_HEAD_DIM
N_BRANCH = 3
SPLIT_IDX = [POOL_WIDTH,
             POOL_WIDTH + DA_QK_WIDTH,
             POOL_WIDTH + 2 * DA_QK_WIDTH,
             POOL_WIDTH + 2 * DA_QK_WIDTH + DA_WIDTH,
             POOL_WIDTH + 2 * DA_QK_WIDTH + DA_WIDTH + MEM_WIDTH]
IN_COLS = SPLIT_IDX[-1] + N_BRANCH * D_MODEL
N_GROUPS = 4
EXPERTS_PER_GROUP = 4
N_EXPERTS = N_GROUPS * EXPERTS_PER_GROUP
TOP_K_INNER = 2
EXPERT_HIDDEN = D_MODEL // 2
MOE_BLOCK = 512

kernel_name = "hybrid_pool_diffattn_mem_hmoe"


def rmsnorm(x, g):
    xf = x.astype(jnp.float32)
    r = lax.rsqrt(jnp.mean(xf * xf, axis=-1, keepdims=True) + RMS_EPS)
    return (xf * r).astype(x.dtype) * g


def pool_mixer(u, w_group, scale):
    b, s, _ = u.shape
    ug = u.reshape(b, s, len(POOL_WINDOWS), POOL_GROUP).astype(jnp.float32)
    c = jnp.pad(jnp.cumsum(ug, axis=1), ((0, 0), (1, 0), (0, 0), (0, 0)))
    t = jnp.arange(s)
    means = []
    for gi, w in enumerate(POOL_WINDOWS):
        lo = jnp.maximum(t + 1 - w, 0)
        cnt = (t + 1 - lo).astype(jnp.float32)
        win_sum = c[:, 1:, gi] - c[:, lo, gi]
        means.append(win_sum / cnt[None, :, None])
    pooled = (jnp.stack(means, axis=2) - ug).astype(u.dtype)
    mixed = jnp.einsum('bsgc,gcd->bsgd', pooled, w_group)
    return mixed.reshape(b, s, POOL_WIDTH) * scale


def diff_attention(q, k, v, lam, subln_g, lambda_init):
    b, s = q.shape[0], q.shape[1]
    nb = s // Q_BLOCK
    scale = DA_HEAD_DIM ** -0.5
    qb = q.reshape(b, nb, Q_BLOCK, DA_HEADS, 2, DA_HEAD_DIM).transpose(1, 0, 2, 3, 4, 5)
    k_pos = jnp.arange(s)

    def one_block(args):
        q_blk, i = args
        scores = jnp.einsum('bqhmd,bkhmd->bhmqk', q_blk, k).astype(jnp.float32) * scale
        q_pos = i * Q_BLOCK + jnp.arange(Q_BLOCK)
        causal = k_pos[None, :] <= q_pos[:, None]
        scores = jnp.where(causal, scores, -jnp.inf)
        p = jax.nn.softmax(scores, axis=-1)
        a = p[:, :, 0] - lam * p[:, :, 1]
        return jnp.einsum('bhqk,bkhv->bqhv', a.astype(v.dtype), v)

    o = lax.map(one_block, (qb, jnp.arange(nb)))
    o = o.transpose(1, 0, 2, 3, 4).reshape(b, s, DA_HEADS, DA_V_DIM)
    o = rmsnorm(o, subln_g) * (1.0 - lambda_init)
    return o.reshape(b, s, DA_WIDTH)


def memory_attention(q, mem_n, w_kv):
    b, m, _ = mem_n.shape
    kv = mem_n @ w_kv
    mk = kv[..., :MEM_WIDTH].reshape(b, m, MEM_HEADS, MEM_HEAD_DIM)
    mv = kv[..., MEM_WIDTH:].reshape(b, m, MEM_HEADS, MEM_HEAD_DIM)
    scores = jnp.einsum('bshd,bmhd->bhsm', q, mk).astype(jnp.float32) * (MEM_HEAD_DIM ** -0.5)
    p = jax.nn.softmax(scores, axis=-1)
    o = jnp.einsum('bhsm,bmhd->bshd', p.astype(mv.dtype), mv)
    return o.reshape(q.shape[0], q.shape[1], MEM_WIDTH)


def hier_moe(h, w_rg, b_rg, w_re, b_re, w_gate, w_up, w_down):
    b, s, d = h.shape
    t = h.reshape(-1, d)
    n_tok = t.shape[0]
    g_prob = jax.nn.softmax((t @ w_rg).astype(jnp.float32) + b_rg, axis=-1)
    g_p, g_idx = lax.top_k(g_prob, 1)
    e_logits = jnp.einsum('td,dge->tge', t, w_re).astype(jnp.float32) + b_re
    e_sel = jnp.take_along_axis(e_logits, g_idx[:, :, None], axis=1)[:, 0]
    e_p, e_idx = lax.top_k(jax.nn.softmax(e_sel, axis=-1), TOP_K_INNER)
    weights = g_p * (e_p / jnp.sum(e_p, axis=-1, keepdims=True))
    expert_id = g_idx * EXPERTS_PER_GROUP + e_idx

    flat_e = expert_id.reshape(-1)
    flat_w = weights.reshape(-1)
    flat_tok = jnp.repeat(jnp.arange(n_tok), TOP_K_INNER)
    n_assign = flat_e.shape[0]
    order = jnp.argsort(flat_e)
    se, stok, sw = flat_e[order], flat_tok[order], flat_w[order]
    counts = jnp.zeros((N_EXPERTS,), jnp.int32).at[flat_e].add(1)
    padded = ((counts + MOE_BLOCK - 1) // MOE_BLOCK) * MOE_BLOCK
    start = jnp.cumsum(counts) - counts
    pend = jnp.cumsum(padded)
    pstart = pend - padded
    dest = pstart[se] + jnp.arange(n_assign) - start[se]
    n_blocks = -(-n_assign // MOE_BLOCK) + N_EXPERTS
    n_rows = n_blocks * MOE_BLOCK
    rows = jnp.zeros((n_rows, d), t.dtype).at[dest].set(t[stok])
    block_start = jnp.arange(n_blocks) * MOE_BLOCK
    block_expert = jnp.minimum(jnp.searchsorted(pend, block_start, side='right'), N_EXPERTS - 1)

    def expert_block(args):
        xb, e = args
        hid = jax.nn.silu(xb @ w_gate[e]) * (xb @ w_up[e])
        return hid @ w_down[e]

    y_rows = lax.map(expert_block, (rows.reshape(n_blocks, MOE_BLOCK, d), block_expert)).reshape(n_rows, d)
    contrib = (y_rows[dest] * sw[:, None]).astype(t.dtype)
    out = jnp.zeros((n_tok, d), t.dtype).at[stok].add(contrib)
    return out.reshape(b, s, d)


def setup_inputs(seed: int = 0) -> dict:
    key = jax.random.key(seed)
    ks = jax.random.split(key, 32)
    f32 = jnp.float32
    D, L = D_MODEL, DEPTH

    def nrm(k, shape, fan_in):
        return jax.random.normal(k, shape, f32) * (fan_in ** -0.5)

    def gain(k, shape):
        return 1.0 + 0.02 * jax.random.normal(k, shape, f32)

    return {
        "x": jax.random.normal(ks[0], (BATCH, SEQ, D), f32),
        "mem": jax.random.normal(ks[1], (BATCH, N_MEM, D), f32),
        "norm_mix_g": gain(ks[2], (L, D)),
        "w_in": nrm(ks[3], (L, D, IN_COLS), D),
        "pool_w": nrm(ks[4], (L, len(POOL_WINDOWS), POOL_GROUP, POOL_GROUP), POOL_GROUP),
        "pool_scale": gain(ks[5], (L, POOL_WIDTH)),
        "lam_q1": 0.1 * jax.random.normal(ks[6], (L, DA_HEAD_DIM), f32),
        "lam_k1": 0.1 * jax.random.normal(ks[7], (L, DA_HEAD_DIM), f32),
        "lam_q2": 0.1 * jax.random.normal(ks[8], (L, DA_HEAD_DIM), f32),
        "lam_k2": 0.1 * jax.random.normal(ks[9], (L, DA_HEAD_DIM), f32),
        "subln_g": gain(ks[10], (L, DA_V_DIM)),
        "norm_mem_g": gain(ks[11], (L, D)),
        "w_mem_kv": nrm(ks[12], (L, D, 2 * MEM_WIDTH), D),
        "p_pool": nrm(ks[13], (L, POOL_WIDTH, D), POOL_WIDTH),
        "p_diff": nrm(ks[14], (L, DA_WIDTH, D), DA_WIDTH),
        "p_mem": nrm(ks[15], (L, MEM_WIDTH, D), MEM_WIDTH),
        "w_o": nrm(ks[16], (L, D, D), D),
        "norm_ffn_g": gain(ks[17], (L, D)),
        "w_router_group": nrm(ks[18], (L, D, N_GROUPS), D),
        "b_router_group": 0.01 * jax.random.normal(ks[19], (L, N_GROUPS), f32),
        "w_router_expert": nrm(ks[20], (L, D, N_GROUPS, EXPERTS_PER_GROUP), D),
        "b_router_expert": 0.01 * jax.random.normal(ks[21], (L, N_GROUPS, EXPERTS_PER_GROUP), f32),
        "w_expert_gate": nrm(ks[22], (L, N_EXPERTS, D, EXPERT_HIDDEN), D),
        "w_expert_up": nrm(ks[23], (L, N_EXPERTS, D, EXPERT_HIDDEN), D),
        "w_expert_down": nrm(ks[24], (L, N_EXPERTS, EXPERT_HIDDEN, D), EXPERT_HIDDEN),
        "final_g": gain(ks[25], (D,)),
    }


def reference(x, mem, norm_mix_g, w_in, pool_w, pool_scale, lam_q1, lam_k1, lam_q2, lam_k2,
              subln_g, norm_mem_g, w_mem_kv, p_pool, p_diff, p_mem, w_o, norm_ffn_g,
              w_router_group, b_router_group, w_router_expert, b_router_expert,
              w_expert_gate, w_expert_up, w_expert_down, final_g):
    b, s, d = x.shape
    for l in range(DEPTH):
        lambda_init = 0.8 - 0.6 * math.exp(-0.3 * l)
        h = rmsnorm(x, norm_mix_g[l])
        proj = h @ w_in[l]
        u_pool, dq, dk, dv, mq, gates = jnp.split(proj, SPLIT_IDX, axis=-1)
        gates = jax.nn.sigmoid(gates.reshape(b, s, N_BRANCH, d))

        pool_out = pool_mixer(u_pool, pool_w[l], pool_scale[l])

        lam = (jnp.exp(jnp.sum(lam_q1[l] * lam_k1[l]).astype(jnp.float32))
               - jnp.exp(jnp.sum(lam_q2[l] * lam_k2[l]).astype(jnp.float32)) + lambda_init)
        diff_out = diff_attention(dq.reshape(b, s, DA_HEADS, 2, DA_HEAD_DIM),
                                  dk.reshape(b, s, DA_HEADS, 2, DA_HEAD_DIM),
                                  dv.reshape(b, s, DA_HEADS, DA_V_DIM),
                                  lam, subln_g[l], lambda_init)

        mem_n = rmsnorm(mem, norm_mem_g[l])
        mem_out = memory_attention(mq.reshape(b, s, MEM_HEADS, MEM_HEAD_DIM), mem_n, w_mem_kv[l])

        merged = (gates[:, :, 0] * (pool_out @ p_pool[l])
                  + gates[:, :, 1] * (diff_out @ p_diff[l])
                  + gates[:, :, 2] * (mem_out @ p_mem[l]))
        x = x + merged @ w_o[l]
        x = x + hier_moe(rmsnorm(x, norm_ffn_g[l]), w_router_group[l], b_router_group[l],
                         w_router_expert[l], b_router_expert[l],
                         w_expert_gate[l], w_expert_up[l], w_expert_down[l])
    return rmsnorm(x, final_g)
```

```python
import contextlib
import numpy as np
import concourse.bass as bass
import concourse.mybir as mybir
from concourse.bass_utils import run_bass_kernel_spmd

F32 = mybir.dt.float32
BF16 = mybir.dt.bfloat16
I32 = mybir.dt.int32
AF = mybir.ActivationFunctionType
ALU = mybir.AluOpType
AX = mybir.AxisListType

ENGS = ("pe", "act", "dve", "pool", "sp")


class _Op:
    __slots__ = ("eng", "fn", "deps", "dma_key", "dma_val", "need_inc", "cnt", "dma_waits", "barrier")


class Sched:
    def __init__(self, nc):
        self.nc = nc
        self.ops = []
        self.last_w = {}
        self.readers = {}
        self.dma_cnt = {}
        self.all_dma_keys = []
        self.last_compute = {}

    def _add(self, eng, fn, reads, writes, dma_key=None):
        op = _Op()
        op.eng = eng
        op.fn = fn
        op.dma_key = dma_key
        op.need_inc = False
        op.cnt = 0
        op.barrier = False
        idx = len(self.ops)
        deps = set()
        for r in reads:
            w = self.last_w.get(r)
            if w is not None:
                deps.add(w)
        for r in writes:
            w = self.last_w.get(r)
            if w is not None:
                deps.add(w)
            for rd in self.readers.get(r, ()):
                deps.add(rd)
        op.deps = deps
        op.dma_waits = {}
        for d in deps:
            dop = self.ops[d]
            if dop.dma_key is not None:
                k = dop.dma_key
                op.dma_waits[k] = max(op.dma_waits.get(k, 0), self.dma_cnt[k])
        if dma_key is not None:
            if dma_key not in self.dma_cnt:
                self.dma_cnt[dma_key] = 0
                self.all_dma_keys.append(dma_key)
            self.dma_cnt[dma_key] += 16
            op.dma_val = self.dma_cnt[dma_key]
        else:
            self.last_compute[eng] = idx
        self.ops.append(op)
        for r in reads:
            self.readers.setdefault(r, []).append(idx)
        for r in writes:
            self.last_w[r] = idx
            self.readers[r] = []
        return idx

    def op(self, eng, fn, reads=(), writes=()):
        return self._add(eng, fn, tuple(reads), tuple(writes), None)

    def dma(self, eng, fn, key, reads=(), writes=()):
        return self._add(eng, fn, tuple(reads), tuple(writes), key)

    def barrier(self):
        lc = dict(self.last_compute)
        dw = dict(self.dma_cnt)
        for e in ENGS:
            op = _Op()
            op.eng = e
            op.fn = None
            op.dma_key = None
            op.need_inc = False
            op.cnt = 0
            op.barrier = True
            op.deps = set(lc.values())
            op.dma_waits = dict(dw)
            self.ops.append(op)
        self.last_w = {}
        self.readers = {}

    def emit(self, final_eng="sp"):
        nc = self.nc
        ops = self.ops
        for op in ops:
            for d in op.deps:
                dop = ops[d]
                if dop.dma_key is None:
                    if dop.eng == "pe" and op.eng == "pe" and not op.barrier:
                        continue
                    dop.need_inc = True
        cnt = {e: 0 for e in ENGS}
        for op in ops:
            if op.dma_key is None and op.need_inc:
                cnt[op.eng] += 1
                op.cnt = cnt[op.eng]
        per_eng = {e: [] for e in ENGS}
        for op in ops:
            per_eng[op.eng].append(op)
        with contextlib.ExitStack() as st:
            esem = {e: st.enter_context(nc.semaphore("sem_" + e)) for e in ENGS}
            dsem = {k: st.enter_context(nc.semaphore("dsem_%d" % i)) for i, k in enumerate(self.all_dma_keys)}
            block = st.enter_context(nc.Block())

            def run_engine(ename, engine):
                waited = {}
                for op in per_eng[ename]:
                    need = {}
                    for d in op.deps:
                        dop = ops[d]
                        if dop.dma_key is not None:
                            continue
                        if dop.eng == "pe" and ename == "pe" and not op.barrier:
                            continue
                        s = ("e", dop.eng)
                        need[s] = max(need.get(s, 0), dop.cnt)
                    for k, v in op.dma_waits.items():
                        s = ("d", k)
                        need[s] = max(need.get(s, 0), v)
                    for s, v in need.items():
                        if waited.get(s, 0) >= v:
                            continue
                        waited[s] = v
                        sem = esem[s[1]] if s[0] == "e" else dsem[s[1]]
                        engine.wait_ge(sem, v)
                    if op.fn is None:
                        continue
                    ins = op.fn(engine)
                    if op.dma_key is not None:
                        ins.then_inc(dsem[op.dma_key], 16)
                    elif op.need_inc:
                        ins.then_inc(esem[ename], 1)
                if ename == final_eng:
                    for k in self.all_dma_keys:
                        v = self.dma_cnt[k]
                        if waited.get(("d", k), 0) < v:
                            engine.wait_ge(dsem[k], v)

            block.tensor(lambda e: run_engine("pe", e))
            block.scalar(lambda e: run_engine("act", e))
            block.vector(lambda e: run_engine("dve", e))
            block.gpsimd(lambda e: run_engine("pool", e))
            block.sync(lambda e: run_engine("sp", e))


class Arena:
    def __init__(self, ap):
        self.ap = ap
        self.off = 0
        self.cap = ap.shape[1] * 2

    def alloc(self, shape, dt):
        assert shape[0] == 128
        n = 1
        for s in shape[1:]:
            n *= s
        esz = 2 if dt == BF16 else 4
        nb = n * esz
        start = (self.off + 31) // 32 * 32
        self.off = start + nb
        assert self.off <= self.cap, ("arena overflow", self.off, self.cap)
        a = self.ap[:, start // 2:(start + nb) // 2]
        if dt != BF16:
            a = a.bitcast(dt)
        if len(shape) > 2:
            names = " ".join("d%d" % i for i in range(len(shape) - 1))
            kw = {"d%d" % i: shape[i + 1] for i in range(len(shape) - 1)}
            a = a.rearrange("p (%s) -> p %s" % (names, names), **kw)
        return a


D = 1024
NTOK = 8192
NT = 64
GS = 512
NGRP = 16
NBLK = 48
NROWS = NBLK * 512
EPS = 1e-6
LAMBDA_INIT = 0.8 - 0.6 * 1.0
BIG = 1.0e4


def build_nc(stage=9):
    nc = bass.Bass("TRN2", target_bir_lowering=False)

    def din(name, shape, dt=F32):
        return nc.dram_tensor(name, shape, dt, kind="ExternalInput").ap()

    x = din("x", [NTOK, D])
    mem = din("mem", [1024, D])
    gmix = din("gmix", [128, 8])
    gmem = din("gmem", [1, D])
    gffn = din("gffn", [1, D])
    gfin = din("gfin", [1, D])
    w_in_a = din("w_in_a", [D, 2048])
    w_fc = din("w_fc", [8, 128, 32 * 128])
    pool_w = din("pool_w", [4, 64, 64])
    pscale = din("pscale", [128, 2])
    lamv = din("lamv", [1, 256])
    subg = din("subg", [1, 128])
    wkv = din("wkv", [D, 512])
    w_o = din("w_o", [D, D])
    w_r = din("w_r", [D, 20])
    b_r = din("b_r", [1, 20])
    w_gate = din("w_gate", [16 * 1024, 512])
    w_up = din("w_up", [16 * 1024, 512])
    w_down = din("w_down", [16 * 512, 1024])
    out = nc.dram_tensor("out", [NTOK, D], F32, kind="ExternalOutput").ap()
    x1s = nc.dram_tensor("x1s", [NTOK, D], F32, kind="Internal").ap()
    h2s = nc.dram_tensor("h2s", [NTOK, D], BF16, kind="Internal").ap()
    rows = nc.dram_tensor("rows", [NROWS, D], BF16, kind="Internal").ap()
    yrows = nc.dram_tensor("yrows", [NROWS, D], BF16, kind="Internal").ap()
    mks = nc.dram_tensor("mks", [4, 128, 512], BF16, kind="Internal").ap()
    mvs = nc.dram_tensor("mvs", [4, 128, 528], BF16, kind="Internal").ap()
    wfc_bf = nc.dram_tensor("wfc_bf", [8, 128, 4096], BF16, kind="Internal").ap()
    wexp = [nc.dram_tensor("wexp%d" % i, [16 * 128, 4096], BF16, kind="Internal").ap() for i in range(3)]

    S = Sched(nc)
    with contextlib.ExitStack() as st:
        arena_t = st.enter_context(nc.sbuf_tensor("arena", [128, 106200], BF16))
        ps = st.enter_context(nc.psum_tensor("ps", [128, 8, 512], F32))
        A = Arena(arena_t[:, :])

        def PB(b):
            return ("pb", b)

        bank_rr = [0]

        def next_bank(pool=(0, 1, 2, 3, 4, 5, 6, 7)):
            b = pool[bank_rr[0] % len(pool)]
            bank_rr[0] += 1
            return b

        flip = [0]

        def evac_eng():
            flip[0] ^= 1
            return "act" if flip[0] else "dve"

        def copy_op(eng, out_ap, in_ap, reads, writes):
            if eng == "act":
                S.op("act", lambda e: e.activation(out=out_ap, in_=in_ap, func=AF.Copy), reads, writes)
            elif eng == "dve":
                S.op("dve", lambda e: e.tensor_copy(out_ap, in_ap), reads, writes)
            else:
                S.op("pool", lambda e: e.tensor_copy(out_ap, in_ap), reads, writes)

        ident_bf = A.alloc([128, 128], BF16)
        ident_f = A.alloc([128, 128], F32)
        maskb = A.alloc([128, 128], BF16)
        U_bf = A.alloc([128, 128], BF16)
        ones_bf = A.alloc([128, 128], BF16)
        logits = A.alloc([128, NT, 20], F32)
        dhi_i = A.alloc([128, NT], I32)
        dlo_i = A.alloc([128, NT], I32)
        whi = A.alloc([128, NT], F32)
        wlo = A.alloc([128, NT], F32)
        idxe_i = A.alloc([128, NBLK], I32)
        persist_off = A.off

        S.op("pool", lambda e: e.memset(ident_bf, 1.0), writes=["ident_bf"])
        S.op("pool", lambda e: e.affine_select(out=ident_bf, in_=ident_bf, pattern=[[-1, 128]], compare_op=ALU.is_equal, fill=0.0, base=0, channel_multiplier=1), reads=["ident_bf"], writes=["ident_bf"])
        S.op("pool", lambda e: e.memset(ident_f, 1.0), writes=["ident_f"])
        S.op("pool", lambda e: e.affine_select(out=ident_f, in_=ident_f, pattern=[[-1, 128]], compare_op=ALU.is_equal, fill=0.0, base=0, channel_multiplier=1), reads=["ident_f"], writes=["ident_f"])
        S.op("pool", lambda e: e.memset(maskb, 0.0), writes=["maskb"])
        S.op("pool", lambda e: e.affine_select(out=maskb, in_=maskb, pattern=[[1, 128]], compare_op=ALU.is_ge, fill=-30000.0, base=0, channel_multiplier=-1), reads=["maskb"], writes=["maskb"])
        S.op("pool", lambda e: e.memset(U_bf, 1.0), writes=["U_bf"])
        S.op("pool", lambda e: e.affine_select(out=U_bf, in_=U_bf, pattern=[[1, 128]], compare_op=ALU.is_gt, fill=0.0, base=0, channel_multiplier=-1), reads=["U_bf"], writes=["U_bf"])
        S.op("pool", lambda e: e.memset(ones_bf, 1.0), writes=["ones_bf"])

        w_in_sb = A.alloc([128, 8, 2048], BF16)
        wo_sb = A.alloc([128, 8, D], BF16)
        wr_sb = A.alloc([128, 8, 20], F32)
        br_b = A.alloc([128, 20], F32)
        gmixT = A.alloc([128, 8], F32)
        gffn_b = A.alloc([128, D], F32)
        wbd = A.alloc([128, 2, 128], BF16)
        pscale_s = A.alloc([128, 2], F32)
        subg_b = A.alloc([128, 128], F32)
        lam_b = A.alloc([128, 256], F32)
        lam_t = A.alloc([128, 4], F32)
        invw = A.alloc([128, 2], F32)
        invc0 = A.alloc([128, 2, 16], F32)
        wg_s = [A.alloc([128, 32, 128], BF16) for _ in range(2)]
        x_sb = A.alloc([128, 4, D], F32)
        junk_f = A.alloc([128, D], F32)
        junk_b = A.alloc([128, D], BF16)
        h_tm = [A.alloc([128, D], BF16) for _ in range(2)]
        hT = A.alloc([128, 8, GS], BF16)
        ubuf = A.alloc([128, 2, 528], F32)
        sA = A.alloc([128, 2, 528], F32)
        sB = A.alloc([128, 2, 528], F32)
        sAf = sA.rearrange("p a b -> p (a b)")
        sBf = sB.rearrange("p a b -> p (a b)")
        mtmp = [sAf[:, 0:512], sAf[:, 512:1024], sBf[:, 0:512]]
        mkey = ["sA", "sA", "sB"]
        pooled = A.alloc([128, 2, GS], BF16)
        pool_outT = A.alloc([128, 2, GS], BF16)
        alias_off = A.off
        qT = A.alloc([128, 4, GS], BF16)
        kT = A.alloc([128, 4, 2048], BF16)
        V_aug = A.alloc([128, 16, 4, 130], BF16)
        alias_end = A.off
        mqT = A.alloc([128, 2, GS], BF16)
        mkT_all = A.alloc([128, 1, 2, 256], BF16)
        mv_all = A.alloc([128, 1, 2, 4, 66], BF16)
        pT_all = A.alloc([128, 2, 2, GS], BF16)
        pT = [pT_all[:, 0, :, :], pT_all[:, 1, :, :]]

        def PTK(i):
            return [("pT", i, 0), ("pT", i, 1)]
        h2T_f = pT_all.rearrange("p a b c -> p (a b c)").bitcast(F32).rearrange("p (k t) -> p k t", k=8)
        dall_raw = A.alloc([128, 4 * 4 * 128 * 2], BF16)
        dall = dall_raw.bitcast(F32).rearrange("p (a b d) -> p a b d", a=4, b=4)
        mergedT = dall_raw.rearrange("p (k t) -> p k t", k=8)
        t1buf = A.alloc([128, 128], F32)
        rz = A.alloc([128, 8], F32)
        ss16 = A.alloc([128, 16], F32)
        diff_tm = [A.alloc([128, 4, 128], BF16)] * 2
        diff_outT = A.alloc([128, 4, GS], BF16)
        mem_tm = A.alloc([128, 4, 256], BF16)
        rzm = A.alloc([128, 4, 1], F32)
        mem_outT = A.alloc([128, 2, GS], BF16)
        gate_sb = [A.alloc([128, 3, GS], BF16)] * 2
        ssx = A.alloc([128, 4], F32)
        ss2 = A.alloc([128, 4], F32)
        h2b = [A.alloc([128, D], BF16), junk_b]
        h2f = junk_f
        stg = [A.alloc([128, 2048], BF16) for _ in range(2)]
        p1_end = A.off

        w_in_v = w_in_a.rearrange("(k p) n -> p k n", p=128)
        S.dma("sp", lambda e: e.dma_start(out=wr_sb, in_=w_r.rearrange("(k p) n -> p k n", p=128)), "wr_sb", writes=["wr_sb"])
        S.dma("sp", lambda e: e.dma_start(out=br_b, in_=b_r.partition_broadcast(128)), "br_b", writes=["br_b"])
        S.dma("sp", lambda e: e.dma_start(out=gmixT, in_=gmix), "gmixT", writes=["gmixT"])
        S.dma("sp", lambda e: e.dma_start(out=gffn_b, in_=gffn.partition_broadcast(128)), "gffn_b", writes=["gffn_b"])
        S.dma("sp", lambda e: e.dma_start(out=pscale_s, in_=pscale), "pscale_s", writes=["pscale_s"])
        S.dma("sp", lambda e: e.dma_start(out=subg_b, in_=subg.partition_broadcast(128)), "subg_b", writes=["subg_b"])
        S.dma("sp", lambda e: e.dma_start(out=lam_b, in_=lamv.partition_broadcast(128)), "lam_b", writes=["lam_b"])
        S.op("pool", lambda e: e.memset(wbd, 0.0), writes=["wbd"])
        for gi in range(4):
            c, hb = gi // 2, (gi % 2) * 64
            S.dma("pool", (lambda gi, c, hb: lambda e: e.dma_start(out=wbd[hb:hb + 64, c, hb:hb + 64], in_=pool_w[gi]))(gi, c, hb), "wbd", reads=["wbd"], writes=["wbd"])
        S.op("dve", lambda e: e.tensor_scalar(out=subg_b, in0=subg_b, scalar1=1.0 - LAMBDA_INIT, scalar2=None, op0=ALU.mult), reads=["subg_b"], writes=["subg_b"])
        S.op("dve", lambda e: e.tensor_tensor(out=lam_b[:, 0:64], in0=lam_b[:, 0:64], in1=lam_b[:, 64:128], op=ALU.mult), reads=["lam_b"], writes=["lam_b"])
        S.op("dve", lambda e: e.tensor_tensor(out=lam_b[:, 128:192], in0=lam_b[:, 128:192], in1=lam_b[:, 192:256], op=ALU.mult), reads=["lam_b"], writes=["lam_b"])
        S.op("dve", lambda e: e.tensor_reduce(out=lam_t[:, 0:1], in_=lam_b[:, 0:64], axis=AX.X, op=ALU.add), reads=["lam_b"], writes=["lam_t"])
        S.op("dve", lambda e: e.tensor_reduce(out=lam_t[:, 1:2], in_=lam_b[:, 128:192], axis=AX.X, op=ALU.add), reads=["lam_b"], writes=["lam_t"])
        S.op("act", lambda e: e.activation(out=lam_t[:, 0:2], in_=lam_t[:, 0:2], func=AF.Exp), reads=["lam_t"], writes=["lam_t"])
        S.op("dve", lambda e: e.tensor_tensor(out=lam_t[:, 2:3], in0=lam_t[:, 0:1], in1=lam_t[:, 1:2], op=ALU.subtract), reads=["lam_t"], writes=["lam_t"])
        S.op("dve", lambda e: e.tensor_scalar(out=lam_t[:, 3:4], in0=lam_t[:, 2:3], scalar1=LAMBDA_INIT, scalar2=None, op0=ALU.add), reads=["lam_t"], writes=["lam_t"])
        lam_ap = lam_t[:, 3:4]
        wins = {(0, 0): 2.0, (1, 0): 4.0, (0, 1): 8.0, (1, 1): 16.0}
        for (hh, c), w in wins.items():
            S.op("pool", (lambda hh, c, w: lambda e: e.memset(invw[hh * 64:hh * 64 + 64, c:c + 1], 1.0 / w))(hh, c, w), reads=["invw"], writes=["invw"])
            for j in range(16):
                pass
        for (hh, c), w in wins.items():
            for j in range(16):
                v = 1.0 / min(j + 1.0, w)
                if j + 1 >= w:
                    S.op("pool", (lambda hh, c, j, v: lambda e: e.memset(invc0[hh * 64:hh * 64 + 64, c, j:16], v))(hh, c, j, v), reads=["invc0"], writes=["invc0"])
                    break
                S.op("pool", (lambda hh, c, j, v: lambda e: e.memset(invc0[hh * 64:hh * 64 + 64, c, j:j + 1], v))(hh, c, j, v), reads=["invc0"], writes=["invc0"])
        S.op("pool", lambda e: e.memset(mv_all[:, :, :, :, 64:66], 1.0), writes=["mv_all"])

        def rstd_from_ss(ss_ap, n, key):
            S.op("dve", lambda e: e.tensor_scalar(out=ss_ap, in0=ss_ap, scalar1=1.0 / n, scalar2=EPS, op0=ALU.mult, op1=ALU.add), reads=[key], writes=[key])
            S.op("act", lambda e: e.activation(out=ss_ap, in_=ss_ap, func=AF.Sqrt), reads=[key], writes=[key])
            S.op("dve", lambda e: e.reciprocal(out=ss_ap, in_=ss_ap), reads=[key], writes=[key])

        def sumsq(src_ap, dst_col, src_keys, dst_key):
            S.op("act", lambda e: e.activation(out=junk_b, in_=src_ap, func=AF.Square, accum_out=dst_col), reads=list(src_keys) + [dst_key], writes=["junk_b", dst_key])

        def transpose8(src_tm, src_key, dst_ap, dst_key, bank, eng):
            pbf = ps[:, bank, :].bitcast(BF16)
            for kc in range(8):
                S.op("pe", (lambda kc: lambda e: e.transpose(pbf[:, kc * 128:(kc + 1) * 128], src_tm[:, kc * 128:(kc + 1) * 128], ident_bf))(kc), reads=[src_key, "ident_bf"], writes=[PB(bank)])
            if eng == "gain":
                S.op("dve", lambda e: e.tensor_tensor(out=dst_ap, in0=pbf.rearrange("p (k t) -> p k t", k=8), in1=gmixT.unsqueeze(2).to_broadcast([128, 8, 128]), op=ALU.mult), reads=[PB(bank), "gmixT"], writes=[dst_key, PB(bank)])
            else:
                copy_op(eng, dst_ap, pbf.rearrange("p (k t) -> p k t", k=8), [PB(bank)], [dst_key, PB(bank)])

        def conv_chunks(ex):
            ch = []
            for mi, wsrc in ((0, w_gate), (1, w_up)):
                for j in range(2):
                    src = wsrc[ex * 1024 + j * 512: ex * 1024 + (j + 1) * 512, :].rearrange("(k p) n -> p k n", p=128)
                    dst = wexp[mi][ex * 128:(ex + 1) * 128, j * 2048:(j + 1) * 2048].rearrange("p (k n) -> p k n", k=4)
                    ch.append((src, dst, 4))
            for j in range(2):
                src = w_down[ex * 512 + j * 256: ex * 512 + (j + 1) * 256, :].rearrange("(k p) n -> p k n", p=128)
                dst = wexp[2][ex * 128:(ex + 1) * 128, j * 2048:(j + 1) * 2048].rearrange("p (k n) -> p k n", k=2)
                ch.append((src, dst, 2))
            return ch

        def conv_step(ch, k):
            n = len(ch)
            if 1 <= k <= n:
                src, dst, kk = ch[k - 1]
                sb_ = stg[(k - 1) % 2]
                S.dma("sp", (lambda dst, sb_, kk: lambda e: e.dma_start(out=dst, in_=sb_.rearrange("p (k n) -> p k n", k=kk)))(dst, sb_, kk), "wexpst", reads=[("stg", (k - 1) % 2)])
            if k < n:
                src, dst, kk = ch[k]
                sb_ = stg[k % 2]
                S.dma("pool", (lambda src, sb_, kk: lambda e: e.dma_start(out=sb_.rearrange("p (k n) -> p k n", k=kk), in_=src))(src, sb_, kk), ("stg", k % 2), writes=[("stg", k % 2)])

        _save_off = A.off
        A.off = alias_off
        wkv_sb = A.alloc([128, 8, 512], BF16)
        gmem_b = A.alloc([128, D], F32)
        mn_tm = A.alloc([128, D], BF16)
        mnT = A.alloc([128, 8, 256], BF16)
        ssm = A.alloc([128, 2], F32)
        mem_sb = A.alloc([128, 2, D], F32)
        assert A.off <= alias_end, (A.off, alias_end)
        A.off = _save_off

        def mem_prologue(b):
            if b == 0:
                S.dma("pool", lambda e: e.dma_start(out=wkv_sb, in_=wkv.rearrange("(k p) n -> p k n", p=128)), "wkv_sb", writes=["wkv_sb"])
                S.dma("sp", lambda e: e.dma_start(out=gmem_b, in_=gmem.partition_broadcast(128)), "gmem_b", writes=["gmem_b"])
            for mt in range(2):
                r0 = b * 256 + mt * 128
                S.dma("sp", (lambda mt, r0: lambda e: e.dma_start(out=mem_sb[:, mt, :], in_=mem[r0:r0 + 128, :]))(mt, r0), ("mem_sb", mt), writes=[("mem_sb", mt)])
                sumsq(mem_sb[:, mt, :], ssm[:, mt:mt + 1], [("mem_sb", mt)], "ssm")
            rstd_from_ss(ssm, float(D), "ssm")
            for mt in range(2):
                S.op("dve", (lambda mt: lambda e: e.scalar_tensor_tensor(out=mn_tm, in0=mem_sb[:, mt, :], scalar=ssm[:, mt:mt + 1], in1=gmem_b, op0=ALU.mult, op1=ALU.mult))(mt), reads=[("mem_sb", mt), "ssm", "gmem_b"], writes=["mn_tm"])
                transpose8(mn_tm, "mn_tm", mnT[:, :, mt * 128:(mt + 1) * 128], "mnT", next_bank(), evac_eng())
            for j in range(2):
                bk = next_bank()
                for kc in range(8):
                    S.op("pe", (lambda j, kc, bk: lambda e: e.matmul(ps[:, bk, 0:256], lhsT=wkv_sb[:, kc, j * 128:(j + 1) * 128], rhs=mnT[:, kc, :], start=(kc == 0), stop=(kc == 7)))(j, kc, bk), reads=["wkv_sb", "mnT"], writes=[PB(bk)])
                copy_op(evac_eng(), mkT_all[:, 0, j, :], ps[:, bk, 0:256], [PB(bk)], ["mkT_all", PB(bk)])
            for mt in range(2):
                bk = next_bank()
                for kc in range(8):
                    S.op("pe", (lambda mt, kc, bk: lambda e: e.matmul(ps[:, bk, 0:256], lhsT=mnT[:, kc, mt * 128:(mt + 1) * 128], rhs=wkv_sb[:, kc, 256:512], start=(kc == 0), stop=(kc == 7)))(mt, kc, bk), reads=["wkv_sb", "mnT"], writes=[PB(bk)])
                copy_op(evac_eng(), mv_all[:, 0, mt, :, 0:64], ps[:, bk, 0:256].rearrange("p (h d) -> p h d", h=4), [PB(bk)], ["mv_all", PB(bk)])
            S.dma("sp", lambda e: e.dma_start(out=mks[b], in_=mkT_all.rearrange("p a j m -> p (a j m)")), "mkst", reads=["mkT_all"], writes=["mks"])
            S.dma("sp", lambda e: e.dma_start(out=mvs[b], in_=mv_all.rearrange("p a m h d -> p (a m h d)")), "mvst", reads=["mv_all"], writes=["mvs"])

        def mem_reload(b):
            S.dma("sp", lambda e: e.dma_start(out=mkT_all.rearrange("p a j m -> p (a j m)"), in_=mks[b]), "mkT_ld", reads=["mks"], writes=["mkT_all"])
            S.dma("sp", lambda e: e.dma_start(out=mv_all.rearrange("p a m h d -> p (a m h d)"), in_=mvs[b]), "mv_ld", reads=["mvs"], writes=["mv_all"])

        allb = tuple(range(8))
        xh = [stg[t // 2][:, (t % 2) * 1024:(t % 2 + 1) * 1024] for t in range(4)]

        def head_load(g):
            tok0 = g * GS
            for t in range(4):
                S.dma("pool", (lambda t: lambda e: e.dma_start(out=xh[t], in_=x[tok0 + t * 128: tok0 + (t + 1) * 128, :]))(t), ("stg", t // 2), writes=[("stg", t // 2)])

        def head_stats(g):
            for t in range(4):
                sumsq(xh[t], ssx[:, t:t + 1], [("stg", t // 2)], "ssx")
            rstd_from_ss(ssx, float(D), "ssx")

        def head_T(g, t):
            hb = h_tm[t % 2]
            hk = ("h_tm", t % 2)
            S.op("dve", lambda e: e.tensor_scalar(out=hb, in0=xh[t], scalar1=ssx[:, t:t + 1], scalar2=None, op0=ALU.mult), reads=[("stg", t // 2), "ssx"], writes=[hk])
            transpose8(hb, hk, hT[:, :, t * 128:(t + 1) * 128], "hT", next_bank(), "gain")

        def x_res_load(g):
            tok0 = g * GS
            for t in range(4):
                S.dma("sp", (lambda t: lambda e: e.dma_start(out=x_sb[:, t, :], in_=x[tok0 + t * 128: tok0 + (t + 1) * 128, :]))(t), ("x_sb", t), writes=[("x_sb", t)])

        def proj_part(g):
            b, gs, tok0 = g // 4, g % 4, g * GS
            if gs == 0:
                S.op("pool", lambda e: e.memset(ubuf[:, :, 0:16], 0.0), reads=["ubuf"], writes=["ubuf"])
            for c in [0, 1, 2, 3, 4, 5, 6, 7, 8, 9, 14, 15]:
                bk = next_bank()
                for kc in range(8):
                    S.op("pe", (lambda c, kc, bk: lambda e: e.matmul(ps[:, bk, :], lhsT=w_in_sb[:, kc, c * 128:(c + 1) * 128], rhs=hT[:, kc, :], start=(kc == 0), stop=(kc == 7)))(c, kc, bk), reads=["w_in_sb", "hT"], writes=[PB(bk)])
                if c < 2:
                    copy_op(evac_eng(), ubuf[:, c, 16:528], ps[:, bk, :], [PB(bk)], ["ubuf", PB(bk)])
                elif c < 6:
                    copy_op(evac_eng(), qT[:, c - 2, :], ps[:, bk, :], [PB(bk)], ["qT", PB(bk)])
                elif c < 10:
                    copy_op(evac_eng(), kT[:, c - 6, gs * GS:(gs + 1) * GS], ps[:, bk, :], [PB(bk)], ["kT", PB(bk)])
                else:
                    copy_op(evac_eng(), mqT[:, c - 14, :], ps[:, bk, :], [PB(bk)], ["mqT", PB(bk)])
            for t in range(4):
                bk = next_bank()
                for kc in range(8):
                    S.op("pe", (lambda t, kc, bk: lambda e: e.matmul(ps[:, bk, :], lhsT=hT[:, kc, t * 128:(t + 1) * 128], rhs=w_in_sb[:, kc, 1280:1792], start=(kc == 0), stop=(kc == 7)))(t, kc, bk), reads=["w_in_sb", "hT"], writes=[PB(bk)])
                copy_op(evac_eng(), V_aug[:, gs * 4 + t, :, 0:128], ps[:, bk, :].rearrange("p (h d) -> p h d", h=4), [PB(bk)], ["V_aug", PB(bk)])

            if g == 0:
                wo_v = w_o.rearrange("(k p) n -> p k n", p=128)
                for kc in range(0, 8, 4):
                    S.dma("pool", (lambda kc: lambda e: e.dma_start(out=wo_sb[:, kc:kc + 4, :], in_=wo_v[:, kc:kc + 4, :]))(kc), "wo_sb", writes=["wo_sb"])
                for fc in range(8):
                    wgs = wg_s[fc % 2]
                    wk = ("wg_s", fc % 2)
                    S.dma("pool", (lambda fc, wgs: lambda e: e.dma_start(out=wgs, in_=w_fc[fc].rearrange("p (k c) -> p k c", k=32)))(fc, wgs), ("wg_conv", fc % 2), writes=[wk])
                    S.dma("sp", (lambda fc, wgs: lambda e: e.dma_start(out=wfc_bf[fc].rearrange("p (k c) -> p k c", k=32), in_=wgs))(fc, wgs), "wfcst", reads=[wk], writes=["wfc_bf"])


        def mid_part(g):
            b, gs, tok0 = g // 4, g % 4, g * GS
            S.op("pool", lambda e: e.tensor_tensor(out=sA[:, :, 1:528], in0=ubuf[:, :, 1:528], in1=ubuf[:, :, 0:527], op=ALU.add), reads=["ubuf"], writes=["sA"])
            S.op("pool", lambda e: e.tensor_tensor(out=sB[:, :, 3:528], in0=sA[:, :, 3:528], in1=sA[:, :, 1:526], op=ALU.add), reads=["sA"], writes=["sB"])

            def pool_fin(src, hh, c):
                p0 = hh * 64
                sl = src[p0:p0 + 64, c, 16:528]
                if gs == 0:
                    S.op("pool", lambda e: e.tensor_tensor(out=sA[p0:p0 + 64, c, 0:16], in0=src[p0:p0 + 64, c, 16:32], in1=invc0[p0:p0 + 64, c, :], op=ALU.mult), reads=["sA", "sB", "invc0"], writes=["sA"])
                S.op("pool", lambda e: e.tensor_scalar(out=sl, in0=sl, scalar1=invw[p0:p0 + 64, c:c + 1], scalar2=None, op0=ALU.mult), reads=["sA", "sB", "invw"], writes=["sA", "sB"])
                S.op("pool", lambda e: e.tensor_tensor(out=pooled[p0:p0 + 64, c, :], in0=sl, in1=ubuf[p0:p0 + 64, c, 16:528], op=ALU.subtract), reads=["sA", "sB", "ubuf"], writes=["pooled"])
                if gs == 0:
                    S.op("pool", lambda e: e.tensor_tensor(out=pooled[p0:p0 + 64, c, 0:16], in0=sA[p0:p0 + 64, c, 0:16], in1=ubuf[p0:p0 + 64, c, 16:32], op=ALU.subtract), reads=["sA", "ubuf"], writes=["pooled"])

            pool_fin(sA, 0, 0)
            pool_fin(sB, 1, 0)
            S.op("pool", lambda e: e.tensor_tensor(out=sA[:, 1, 7:528], in0=sB[:, 1, 7:528], in1=sB[:, 1, 3:524], op=ALU.add), reads=["sB", "sA"], writes=["sA"])
            pool_fin(sA, 0, 1)
            S.op("pool", lambda e: e.tensor_tensor(out=sB[64:128, 1, 15:528], in0=sA[64:128, 1, 15:528], in1=sA[64:128, 1, 7:520], op=ALU.add), reads=["sA", "sB"], writes=["sB"])
            pool_fin(sB, 1, 1)
            if gs < 3:
                S.op("pool", lambda e: e.tensor_copy(sA[:, :, 0:16], ubuf[:, :, 512:528]), reads=["ubuf", "sA"], writes=["sA"])
                S.op("pool", lambda e: e.tensor_copy(ubuf[:, :, 0:16], sA[:, :, 0:16]), reads=["sA", "ubuf"], writes=["ubuf"])
            nkc = 4 * gs + 4
            cchunks = []
            if gs == 1:
                cchunks = conv_chunks(4 * b)
            elif gs == 2:
                cchunks = conv_chunks(4 * b + 1)
            elif gs == 3:
                cchunks = conv_chunks(4 * b + 2) + conv_chunks(4 * b + 3)
            nsteps = len(cchunks) + 1 if cchunks else 0
            per_head = (nsteps + 3) // 4
            for h in range(4):
                started = set()

                def acc_ap(m, qt):
                    a = m * 4 + qt
                    return 4 + a // 3, (a % 3) * 130

                def scores(kc, i):
                    j = kc - 4 * gs
                    q0 = max(j, 0) * 128
                    for m in range(2):
                        bk = 2 * i + m
                        S.op("pe", (lambda m, bk, kc, q0, j, h: lambda e: e.matmul(ps[:, bk, q0:GS], lhsT=kT[m * 64:(m + 1) * 64, h, kc * 128:(kc + 1) * 128], rhs=qT[m * 64:(m + 1) * 64, h, q0:GS], start=True, stop=(j < 0)))(m, bk, kc, q0, j, h), reads=["kT", "qT"], writes=[PB(bk)])
                        if j >= 0:
                            S.op("pe", (lambda bk, q0: lambda e: e.matmul(ps[:, bk, q0:q0 + 128], lhsT=ident_bf, rhs=maskb, start=False, stop=True))(bk, q0), reads=["ident_bf", "maskb"], writes=[PB(bk)])
                    segs = [(q0, 256), (256, GS)] if q0 < 256 else [(q0, GS)]
                    for (qa_, qb_) in segs:
                        S.op("act", (lambda i, qa_, qb_: lambda e: e.activation(out=pT[i][:, :, qa_:qb_], in_=ps[:, 2 * i:2 * i + 2, qa_:qb_], func=AF.Exp, scale=0.125))(i, qa_, qb_), reads=[PB(2 * i), PB(2 * i + 1)], writes=[("pT", i, 0 if qa_ < 256 else 1)])

                def pv(kc, i):
                    j = kc - 4 * gs
                    for qt in range(max(j, 0), 4):
                        for m in range(2):
                            bk, off = acc_ap(m, qt)
                            first = bk not in started
                            started.add(bk)
                            last = (kc == 4 * gs + qt)
                            S.op("pe", (lambda m, qt, bk, off, first, last, kc, i, h: lambda e: e.matmul(ps[:, bk, off:off + 129], lhsT=pT[i][:, m, qt * 128:(qt + 1) * 128], rhs=V_aug[:, kc, h, 0:129], start=first, stop=last, skip_group_check=True))(m, qt, bk, off, first, last, kc, i, h), reads=[("pT", i, qt // 2), "V_aug"], writes=[PB(bk)])

                scores(0, 0)
                for kc in range(nkc):
                    if kc + 1 < nkc:
                        scores(kc + 1, (kc + 1) % 2)
                    pv(kc, kc % 2)
                for k in range(h * per_head, min((h + 1) * per_head, nsteps)):
                    conv_step(cchunks, k)
                for m in range(2):
                    for qt in range(4):
                        bk, off = acc_ap(m, qt)
                        a = m * 4 + qt
                        S.op("dve", (lambda bk, off, a: lambda e: e.reciprocal(out=rz[:, a:a + 1], in_=ps[:, bk, off + 128:off + 129]))(bk, off, a), reads=[PB(bk)], writes=["rz", PB(bk)])
                for qt in range(4):
                    bk1, off1 = acc_ap(1, qt)
                    bk0, off0 = acc_ap(0, qt)
                    S.op("dve", (lambda bk1, off1, qt: lambda e: e.tensor_scalar(out=t1buf, in0=ps[:, bk1, off1:off1 + 128], scalar1=rz[:, 4 + qt:5 + qt], scalar2=lam_ap, op0=ALU.mult, op1=ALU.mult))(bk1, off1, qt), reads=[PB(bk1), "rz", "lam_t"], writes=["t1buf", PB(bk1)])
                    S.op("dve", (lambda bk0, off0, qt, h: lambda e: e.scalar_tensor_tensor(out=dall[:, qt, h, :], in0=ps[:, bk0, off0:off0 + 128], scalar=rz[:, qt:qt + 1], in1=t1buf, op0=ALU.mult, op1=ALU.subtract))(bk0, off0, qt, h), reads=[PB(bk0), "rz", "t1buf"], writes=["dall", PB(bk0)])
            for h in range(4):
                j, pb_ = h // 2, (h % 2) * 64
                i = h % 2
                for mc in range(2):
                    bk = mc
                    S.op("pe", (lambda mc, bk, pb_, j: lambda e: e.matmul(ps[:, bk, :], lhsT=mkT_all[pb_:pb_ + 64, 0, j, mc * 128:(mc + 1) * 128], rhs=mqT[pb_:pb_ + 64, j, :], start=True, stop=True))(mc, bk, pb_, j), reads=["mkT_all", "mqT"], writes=[PB(bk)])
                S.op("act", (lambda i: lambda e: e.activation(out=pT[i], in_=ps[:, 0:2, :], func=AF.Exp, scale=0.125))(i), reads=[PB(0), PB(1)], writes=[*PTK(i), PB(0), PB(1)])
                bka = 2 + h
                for qt in range(4):
                    for mc in range(2):
                        S.op("pe", (lambda qt, mc, i, bka, h: lambda e: e.matmul(ps[:, bka, qt * 66:qt * 66 + 65], lhsT=pT[i][:, mc, qt * 128:(qt + 1) * 128], rhs=mv_all[:, 0, mc, h, 0:65], start=(qt == 0 and mc == 0), stop=(mc == 1), skip_group_check=True))(qt, mc, i, bka, h), reads=[*PTK(i), "mv_all"], writes=[PB(bka)])
            dflat = dall.rearrange("p a b d -> p (a b d)")
            for hf in range(2):
                S.op("dve", (lambda hf: lambda e: e.tensor_tensor(out=junk_f, in0=dflat[:, hf * 1024:(hf + 1) * 1024], in1=dflat[:, hf * 1024:(hf + 1) * 1024], op=ALU.mult))(hf), reads=["dall"], writes=["junk_f"])
                S.op("dve", (lambda hf: lambda e: e.tensor_reduce(out=ss16[:, hf * 8:(hf + 1) * 8], in_=junk_f.rearrange("p (a d) -> p a d", d=128), axis=AX.X, op=ALU.add))(hf), reads=["junk_f"], writes=["ss16"])
            rstd_from_ss(ss16, 128.0, "ss16")
            for qt in range(4):
                dt_ = diff_tm[qt % 2]
                dk = ("diff_tm", 0)
                S.op("dve", (lambda qt: lambda e: e.tensor_tensor(out=dall[:, qt, :, :], in0=dall[:, qt, :, :], in1=ss16[:, qt * 4:(qt + 1) * 4].unsqueeze(2).to_broadcast([128, 4, 128]), op=ALU.mult))(qt), reads=["dall", "ss16"], writes=["dall"])
                S.op("dve", (lambda qt, dt_: lambda e: e.tensor_tensor(out=dt_, in0=dall[:, qt, :, :], in1=subg_b.unsqueeze(1).to_broadcast([128, 4, 128]), op=ALU.mult))(qt, dt_), reads=["dall", "subg_b"], writes=[dk])
                pbf = ps[:, 7, :].bitcast(BF16)
                for hh in range(4):
                    S.op("pe", (lambda hh, dt_: lambda e: e.transpose(pbf[:, hh * 128:(hh + 1) * 128], dt_[:, hh, :], ident_bf))(hh, dt_), reads=[dk, "ident_bf"], writes=[PB(7)])
                copy_op(evac_eng(), diff_outT[:, :, qt * 128:(qt + 1) * 128], pbf[:, 0:512].rearrange("p (k t) -> p k t", k=4), [PB(7)], ["diff_outT", PB(7)])

            for h in range(4):
                bka = 2 + h
                psv = ps[:, bka, 0:264].rearrange("p (q c) -> p q c", c=66)
                S.op("dve", (lambda psv: lambda e: e.reciprocal(out=rzm, in_=psv[:, :, 64:65]))(psv), reads=[PB(bka)], writes=["rzm", PB(bka)])
                S.op("dve", (lambda psv, h: lambda e: e.tensor_tensor(out=mem_tm[:, :, h * 64:(h + 1) * 64], in0=psv[:, :, 0:64], in1=rzm.to_broadcast([128, 4, 64]), op=ALU.mult))(psv, h), reads=[PB(bka), "rzm"], writes=["mem_tm", PB(bka)])
            for qt in range(4):
                pbf = ps[:, 7, :].bitcast(BF16)
                for cc in range(2):
                    S.op("pe", (lambda qt, cc: lambda e: e.transpose(pbf[:, cc * 128:(cc + 1) * 128], mem_tm[:, qt, cc * 128:(cc + 1) * 128], ident_bf))(qt, cc), reads=["mem_tm", "ident_bf"], writes=[PB(7)])
                copy_op(evac_eng(), mem_outT[:, :, qt * 128:(qt + 1) * 128], pbf[:, 0:256].rearrange("p (k t) -> p k t", k=2), [PB(7)], ["mem_outT", PB(7)])

            for c in range(2):
                bk = next_bank()
                S.op("pe", (lambda c, bk: lambda e: e.matmul(ps[:, bk, :], lhsT=wbd[:, c, :], rhs=pooled[:, c, :], start=True, stop=True))(c, bk), reads=["wbd", "pooled"], writes=[PB(bk)])
                S.op("act", (lambda c, bk: lambda e: e.activation(out=pool_outT[:, c, :], in_=ps[:, bk, :], func=AF.Copy, scale=pscale_s[:, c:c + 1]))(c, bk), reads=[PB(bk), "pscale_s"], writes=["pool_outT", PB(bk)])


        def merge_part(g, hook):
            b, gs, tok0 = g // 4, g % 4, g * GS
            allb = tuple(range(8))
            for fc in range(8):
                wgs = wg_s[fc % 2]
                wk = ("wg_s", fc % 2)
                S.dma("sp", (lambda fc, wgs: lambda e: e.dma_start(out=wgs, in_=wfc_bf[fc].rearrange("p (k c) -> p k c", k=32)))(fc, wgs), wk, reads=["wfc_bf"], writes=[wk])
                gsb = gate_sb[fc % 2]
                gk = ("gate_sb", 0)
                for br in range(3):
                    bk = next_bank(allb)
                    for kc in range(8):
                        S.op("pe", (lambda br, kc, bk, wgs: lambda e: e.matmul(ps[:, bk, :], lhsT=wgs[:, kc * 3 + br, :], rhs=hT[:, kc, :], start=(kc == 0), stop=(kc == 7)))(br, kc, bk, wgs), reads=[wk, "hT"], writes=[PB(bk)])
                    S.op("act", (lambda br, bk, gsb: lambda e: e.activation(out=gsb[:, br, :], in_=ps[:, bk, :], func=AF.Tanh, scale=0.5))(br, bk, gsb), reads=[PB(bk)], writes=[gk, PB(bk)])
                srcs = [(24, pool_outT, "pool_outT", 2), (26, diff_outT, "diff_outT", 4), (30, mem_outT, "mem_outT", 2)]
                for br, (wbase, act_, akey, nk) in enumerate(srcs):
                    bk = next_bank(allb)
                    for kc in range(nk):
                        S.op("pe", (lambda kc, bk, wbase, act_, nk, wgs: lambda e: e.matmul(ps[:, bk, :], lhsT=wgs[:, wbase + kc, :], rhs=act_[:, kc, :], start=(kc == 0), stop=(kc == nk - 1)))(kc, bk, wbase, act_, nk, wgs), reads=[wk, akey], writes=[PB(bk)])
                    S.op("dve", (lambda br, bk, gsb: lambda e: e.scalar_tensor_tensor(out=mtmp[br], in0=gsb[:, br, :], scalar=1.0, in1=ps[:, bk, :], op0=ALU.add, op1=ALU.mult))(br, bk, gsb), reads=[gk, PB(bk)], writes=[mkey[br], PB(bk)])
                S.op("pool", lambda e: e.tensor_tensor(out=mtmp[0], in0=mtmp[0], in1=mtmp[1], op=ALU.add), reads=["sA"], writes=["sA"])
                S.op("pool", (lambda fc: lambda e: e.tensor_tensor(out=mergedT[:, fc, :], in0=mtmp[0], in1=mtmp[2], op=ALU.add))(fc), reads=["sA", "sB"], writes=["dall"])
                if hook is not None:
                    hook(fc)


        def wo_part(g):
            b, gs, tok0 = g // 4, g % 4, g * GS
            for t in range(4):
                for hf in range(2):
                    bk = next_bank(allb)
                    for kc in range(8):
                        S.op("pe", (lambda t, hf, kc, bk: lambda e: e.matmul(ps[:, bk, :], lhsT=mergedT[:, kc, t * 128:(t + 1) * 128], rhs=wo_sb[:, kc, hf * 512:(hf + 1) * 512], start=(kc == 0), stop=(kc == 7)))(t, hf, kc, bk), reads=["dall", "wo_sb"], writes=[PB(bk)])
                    S.op("dve", (lambda t, hf, bk: lambda e: e.scalar_tensor_tensor(out=x_sb[:, t, hf * 512:(hf + 1) * 512], in0=ps[:, bk, :], scalar=0.5, in1=x_sb[:, t, hf * 512:(hf + 1) * 512], op0=ALU.mult, op1=ALU.add))(t, hf, bk), reads=[PB(bk), ("x_sb", t)], writes=[("x_sb", t), PB(bk)])
                S.dma("sp", (lambda t, tok0: lambda e: e.dma_start(out=x1s[tok0 + t * 128: tok0 + (t + 1) * 128, :], in_=x_sb[:, t, :]))(t, tok0), ("x_sb", t), reads=[("x_sb", t)])
                sumsq(x_sb[:, t, :], ss2[:, t:t + 1], [("x_sb", t)], "ss2")
            if stage != 1:
                rstd_from_ss(ss2, float(D), "ss2")

        def tail_part(g):
            b, gs, tok0 = g // 4, g % 4, g * GS
            for t in range(4):
                T = g * 4 + t
                hb2 = h2b[t % 2]
                hk2 = ("h2b", 0) if t % 2 == 0 else "junk_b"
                S.op("dve", (lambda t: lambda e: e.scalar_tensor_tensor(out=h2f, in0=x_sb[:, t, :], scalar=ss2[:, t:t + 1], in1=gffn_b, op0=ALU.mult, op1=ALU.mult))(t), reads=[("x_sb", t), "ss2", "gffn_b"], writes=["junk_f"])
                S.op("act", (lambda hb2: lambda e: e.activation(out=hb2, in_=h2f, func=AF.Copy))(hb2), reads=["junk_f"], writes=[hk2])
                S.dma("sp", (lambda T, hb2: lambda e: e.dma_start(out=h2s[T * 128:(T + 1) * 128, :], in_=hb2))(T, hb2), hk2, reads=[hk2])
                for half in range(2):
                    bk = next_bank(allb)
                    for kk in range(4):
                        kc = half * 4 + kk
                        S.op("pe", (lambda kc, kk, bk: lambda e: e.transpose(ps[:, bk, kk * 128:(kk + 1) * 128], h2f[:, kc * 128:(kc + 1) * 128], ident_f))(kc, kk, bk), reads=["junk_f", "ident_f"], writes=[PB(bk)])
                    copy_op(evac_eng(), h2T_f[:, half * 4:(half + 1) * 4, :], ps[:, bk, :].rearrange("p (k t) -> p k t", k=4), [PB(bk)], [*PTK(0), *PTK(1), PB(bk)])
                bk = next_bank(allb)
                for kc in range(8):
                    S.op("pe", (lambda kc, bk: lambda e: e.matmul(ps[:, bk, 0:20], lhsT=h2T_f[:, kc, :], rhs=wr_sb[:, kc, :], start=(kc == 0), stop=(kc == 7)))(kc, bk), reads=[*PTK(0), *PTK(1), "wr_sb"], writes=[PB(bk)])
                S.op("dve", (lambda T, bk: lambda e: e.tensor_tensor(out=logits[:, T, :], in0=ps[:, bk, 0:20], in1=br_b, op=ALU.add))(T, bk), reads=[PB(bk), "br_b"], writes=["logits", PB(bk)])


        head_load(0)
        head_stats(0)
        for t in range(4):
            head_T(0, t)
        for g in range(NGRP):
            b, gs = g // 4, g % 4
            if g == 0:
                S.barrier()
                for bb in range(4):
                    mem_prologue(bb)
                    if bb == 0:
                        for kc in range(8):
                            S.dma("pool", (lambda kc: lambda e: e.dma_start(out=w_in_sb[:, kc, :], in_=w_in_v[:, kc, :]))(kc), "w_in_sb", writes=["w_in_sb"])
                S.barrier()
                S.op("pool", lambda e: e.memset(V_aug[:, :, :, 128:130], 1.0), writes=["V_aug"])
            if gs == 0:
                mem_reload(b)
            proj_part(g)
            if g > 0 and stage != 1:
                tail_part(g - 1)
            x_res_load(g)
            mid_part(g)
            nxt = g + 1 < NGRP
            if nxt:
                head_load(g + 1)

            def hook(fc, g=g):
                if fc == 3:
                    head_stats(g + 1)
            merge_part(g, hook if nxt else None)
            if nxt:
                for t in range(4):
                    head_T(g + 1, t)
            wo_part(g)
        if stage != 1:
            tail_part(NGRP - 1)

        S.barrier()
        if stage == 1:
            A.off = persist_off
            cbuf = [A.alloc([128, D], F32) for _ in range(2)]
            for T in range(NT):
                cb = cbuf[T % 2]
                ck = ("cbuf", T % 2)
                S.dma("sp", (lambda T, cb: lambda e: e.dma_start(out=cb, in_=x1s[T * 128:(T + 1) * 128, :]))(T, cb), ck, writes=[ck])
                S.dma("sp", (lambda T, cb: lambda e: e.dma_start(out=out[T * 128:(T + 1) * 128, :], in_=cb))(T, cb), ("co", T % 2), reads=[ck])
            S.emit()
            return nc

        A.off = persist_off
        Lg = logits[:, :, 0:4]
        Le = logits[:, :, 4:20]
        mg = A.alloc([128, NT], F32)
        G = A.alloc([128, NT, 4], F32)
        eg = A.alloc([128, NT, 4], F32)
        gp = A.alloc([128, NT], F32)
        Lp = A.alloc([128, NT, 16], F32)
        m1 = A.alloc([128, NT], F32)
        m2 = A.alloc([128, NT], F32)
        eq1 = A.alloc([128, NT, 16], F32)
        L2 = A.alloc([128, NT, 16], F32)
        Ssel = A.alloc([128, NT, 16], F32)
        ew = A.alloc([128, NT, 16], F32)
        Wt = A.alloc([128, NT, 16], F32)
        S_bf = A.alloc([128, NT, 16], BF16)
        totS = A.alloc([128, NT, 16], F32)
        totB = A.alloc([128, NT, 16], F32)
        rankS = A.alloc([128, NT, 16], F32)
        cntv = A.alloc([128, 16], F32)
        thr_i = A.alloc([128, 16, 32], I32)
        thr = A.alloc([128, 16, 32], F32)
        cmp = A.alloc([128, 16, 32], F32)
        padded = A.alloc([128, 16], F32)
        pst = A.alloc([128, 16], F32)
        pend = A.alloc([128, 16], F32)
        bst_i = A.alloc([128, NBLK], I32)
        bst = A.alloc([128, NBLK], F32)
        cmpb = A.alloc([128, NBLK, 16], F32)
        be = A.alloc([128, NBLK], F32)
        pk_i = A.alloc([128, 8], I32)
        pk = A.alloc([128, 8], F32)
        idxf = A.alloc([128, NBLK], F32)
        dhi = A.alloc([128, NT], F32)
        dlo = A.alloc([128, NT], F32)
        R = ["rt"]

        def dv(fn, extra_r=(), extra_w=()):
            S.op("dve", fn, reads=R + list(extra_r), writes=R + list(extra_w))

        def bc3(ap2, n):
            return ap2.unsqueeze(2).to_broadcast([128, NT, n])

        dv(lambda e: e.tensor_reduce(out=mg, in_=Lg, axis=AX.X, op=ALU.max), extra_r=["logits"])
        dv(lambda e: e.tensor_tensor(out=G, in0=Lg, in1=bc3(mg, 4), op=ALU.is_ge), extra_r=["logits"])
        dv(lambda e: e.tensor_tensor(out=eg, in0=Lg, in1=bc3(mg, 4), op=ALU.subtract), extra_r=["logits"])
        S.op("act", lambda e: e.activation(out=eg, in_=eg, func=AF.Exp), reads=R, writes=R)
        dv(lambda e: e.tensor_reduce(out=gp, in_=eg, axis=AX.X, op=ALU.add))
        dv(lambda e: e.reciprocal(out=gp, in_=gp))
        dv(lambda e: e.tensor_scalar(out=eg, in0=G, scalar1=BIG, scalar2=-BIG, op0=ALU.mult, op1=ALU.add))
        Le4 = Le.rearrange("p t (g x) -> p t g x", g=4)
        Lp4 = Lp.rearrange("p t (g x) -> p t g x", g=4)
        dv(lambda e: e.tensor_tensor(out=Lp4, in0=Le4, in1=eg.unsqueeze(3).to_broadcast([128, NT, 4, 4]), op=ALU.add), extra_r=["logits"])
        dv(lambda e: e.tensor_reduce(out=m1, in_=Lp, axis=AX.X, op=ALU.max))
        dv(lambda e: e.tensor_tensor(out=eq1, in0=Lp, in1=bc3(m1, 16), op=ALU.is_equal))
        dv(lambda e: e.scalar_tensor_tensor(out=L2, in0=eq1, scalar=-BIG, in1=Lp, op0=ALU.mult, op1=ALU.add))
        dv(lambda e: e.tensor_reduce(out=m2, in_=L2, axis=AX.X, op=ALU.max))
        dv(lambda e: e.tensor_tensor(out=Ssel, in0=Lp, in1=bc3(m2, 16), op=ALU.is_ge))
        dv(lambda e: e.tensor_tensor(out=ew, in0=Lp, in1=bc3(m1, 16), op=ALU.subtract))
        S.op("act", lambda e: e.activation(out=ew, in_=ew, func=AF.Exp), reads=R, writes=R)
        dv(lambda e: e.tensor_tensor(out=m2, in0=m2, in1=m1, op=ALU.subtract))
        S.op("act", lambda e: e.activation(out=m2, in_=m2, func=AF.Exp), reads=R, writes=R)
        dv(lambda e: e.tensor_scalar(out=m2, in0=m2, scalar1=1.0, scalar2=None, op0=ALU.add))
        dv(lambda e: e.reciprocal(out=m2, in_=m2))
        dv(lambda e: e.tensor_tensor(out=gp, in0=gp, in1=m2, op=ALU.mult))
        dv(lambda e: e.tensor_tensor(out=Wt, in0=ew, in1=Ssel, op=ALU.mult))
        dv(lambda e: e.tensor_tensor(out=Wt, in0=Wt, in1=bc3(gp, 16), op=ALU.mult))
        dv(lambda e: e.tensor_copy(S_bf, Ssel))
        Sflat = S_bf.rearrange("p t x -> p (t x)")
        for hf in range(2):
            S.op("pe", (lambda hf: lambda e: e.matmul(ps[:, hf, :], lhsT=ones_bf, rhs=Sflat[:, hf * 512:(hf + 1) * 512], start=True, stop=True))(hf), reads=R + ["ones_bf"], writes=[PB(hf)])
            S.op("dve", (lambda hf: lambda e: e.tensor_copy(totS.rearrange("p t x -> p (t x)")[:, hf * 512:(hf + 1) * 512], ps[:, hf, :]))(hf), reads=R + [PB(hf)], writes=R + [PB(hf)])
        for T in range(NT):
            bk = 2 + T // 32
            off = (T % 32) * 16
            S.op("pe", (lambda T, bk, off: lambda e: e.matmul(ps[:, bk, off:off + 16], lhsT=U_bf, rhs=S_bf[:, T, :], start=(T % 32 == 0), stop=(T % 32 == 31), skip_group_check=True))(T, bk, off), reads=R + ["U_bf"], writes=[PB(bk)])
        for hf in range(2):
            S.op("dve", (lambda hf: lambda e: e.tensor_copy(rankS.rearrange("p t x -> p (t x)")[:, hf * 512:(hf + 1) * 512], ps[:, 2 + hf, :]))(hf), reads=R + [PB(2 + hf)], writes=R + [PB(2 + hf)])
        dv(lambda e: e.tensor_copy(L2, totS))
        src, dst = totS, totB
        sh = 1
        while sh < NT:
            dv((lambda src, dst, sh: lambda e: e.tensor_copy(dst[:, 0:sh, :], src[:, 0:sh, :]))(src, dst, sh))
            dv((lambda src, dst, sh: lambda e: e.tensor_tensor(out=dst[:, sh:NT, :], in0=src[:, sh:NT, :], in1=src[:, 0:NT - sh, :], op=ALU.add))(src, dst, sh))
            src, dst = dst, src
            sh *= 2
        incl = src
        dv(lambda e: e.tensor_copy(cntv, incl[:, NT - 1, :]))
        dv(lambda e: e.tensor_tensor(out=incl, in0=incl, in1=L2, op=ALU.subtract))
        S.op("pool", lambda e: e.iota(thr_i, [[0, 16], [512, 32]], base=0, channel_multiplier=0), reads=R, writes=R)
        dv(lambda e: e.tensor_copy(thr, thr_i))
        dv(lambda e: e.tensor_tensor(out=cmp, in0=cntv.unsqueeze(2).to_broadcast([128, 16, 32]), in1=thr, op=ALU.is_gt))
        dv(lambda e: e.tensor_reduce(out=padded, in_=cmp, axis=AX.X, op=ALU.add))
        dv(lambda e: e.tensor_scalar(out=padded, in0=padded, scalar1=512.0, scalar2=None, op0=ALU.mult))
        dv(lambda e: e.memset(pst[:, 0:1], 0.0))
        for ei in range(1, 16):
            dv((lambda ei: lambda e: e.tensor_tensor(out=pst[:, ei:ei + 1], in0=pst[:, ei - 1:ei], in1=padded[:, ei - 1:ei], op=ALU.add))(ei))
        dv(lambda e: e.tensor_tensor(out=pend, in0=pst, in1=padded, op=ALU.add))
        dv(lambda e: e.tensor_tensor(out=incl, in0=incl, in1=rankS, op=ALU.add))
        dv(lambda e: e.tensor_tensor(out=incl, in0=incl, in1=pst.unsqueeze(1).to_broadcast([128, NT, 16]), op=ALU.add))
        dv(lambda e: e.scalar_tensor_tensor(out=incl, in0=incl, scalar=1.0, in1=Ssel, op0=ALU.add, op1=ALU.mult))
        dv(lambda e: e.tensor_scalar(out=incl, in0=incl, scalar1=-1.0, scalar2=None, op0=ALU.add))
        dv(lambda e: e.tensor_reduce(out=dhi, in_=incl, axis=AX.X, op=ALU.max))
        dv(lambda e: e.tensor_tensor(out=eq1, in0=incl, in1=bc3(dhi, 16), op=ALU.is_equal))
        dv(lambda e: e.scalar_tensor_tensor(out=L2, in0=eq1, scalar=-1.0e6, in1=incl, op0=ALU.mult, op1=ALU.add))
        dv(lambda e: e.tensor_reduce(out=dlo, in_=L2, axis=AX.X, op=ALU.max))
        dv(lambda e: e.tensor_tensor(out=L2, in0=eq1, in1=Wt, op=ALU.mult))
        dv(lambda e: e.tensor_reduce(out=whi, in_=L2, axis=AX.X, op=ALU.add), extra_w=["whi"])
        dv(lambda e: e.tensor_tensor(out=L2, in0=Ssel, in1=eq1, op=ALU.subtract))
        dv(lambda e: e.tensor_tensor(out=L2, in0=L2, in1=Wt, op=ALU.mult))
        dv(lambda e: e.tensor_reduce(out=wlo, in_=L2, axis=AX.X, op=ALU.add), extra_w=["wlo"])
        dv(lambda e: e.tensor_copy(dhi_i, dhi), extra_w=["dhi_i"])
        dv(lambda e: e.tensor_copy(dlo_i, dlo), extra_w=["dlo_i"])
        S.op("pool", lambda e: e.iota(bst_i, [[512, NBLK]], base=0, channel_multiplier=0), reads=R, writes=R)
        S.op("pool", lambda e: e.iota(pk_i, [[128, 8]], base=0, channel_multiplier=1), reads=R, writes=R)
        dv(lambda e: e.tensor_copy(bst, bst_i))
        dv(lambda e: e.tensor_copy(pk, pk_i))
        dv(lambda e: e.tensor_tensor(out=cmpb, in0=pend.unsqueeze(1).to_broadcast([128, NBLK, 16]), in1=bst.unsqueeze(2).to_broadcast([128, NBLK, 16]), op=ALU.is_le))
        dv(lambda e: e.tensor_reduce(out=be, in_=cmpb, axis=AX.X, op=ALU.add))
        dv(lambda e: e.tensor_scalar(out=be, in0=be, scalar1=15.0, scalar2=None, op0=ALU.min))
        dv(lambda e: e.scalar_tensor_tensor(out=idxf, in0=be, scalar=128.0, in1=pk[:, 0:1].to_broadcast([128, NBLK]), op0=ALU.mult, op1=ALU.add))
        dv(lambda e: e.tensor_copy(idxe_i, idxf), extra_w=["idxe_i"])
        sbuf2 = [A.alloc([128, D], BF16) for _ in range(4)]
        for T in range(NT):
            sbb = sbuf2[T % 4]
            sk = ("sbuf2", T % 4)
            S.dma("sp", (lambda T, sbb: lambda e: e.dma_start(out=sbb, in_=h2s[T * 128:(T + 1) * 128, :]))(T, sbb), sk, writes=[sk])
            for di, dk_ in ((dhi_i, "dhi_i"), (dlo_i, "dlo_i")):
                S.dma("pool", (lambda T, sbb, di: lambda e: e.indirect_dma_start(out=rows, out_offset=bass.IndirectOffsetOnAxis(ap=di[:, T:T + 1], axis=0), in_=sbb, in_offset=None))(T, sbb, di), ("scat", T % 4), reads=[sk, dk_])
        S.barrier()

        p3_off = A.off
        wg_e = [A.alloc([128, 8, 512], BF16) for _ in range(2)]
        wu_e = [A.alloc([128, 8, 512], BF16) for _ in range(2)]
        wd_e = [A.alloc([128, 4, D], BF16) for _ in range(2)]
        xr = [A.alloc([128, 4, D], BF16) for _ in range(3)]
        XTb = [A.alloc([128, 8, 512], BF16) for _ in range(2)]
        th = [A.alloc([128, 512], F32) for _ in range(2)]
        ga = [A.alloc([128, 512], F32) for _ in range(2)]
        hidT = A.alloc([128, 4, 512], BF16)
        yb = [A.alloc([128, D], BF16) for _ in range(2)]
        allb = tuple(range(8))
        def xr_load(bq):
            iq = bq % 3
            S.dma("sp", lambda e: e.dma_start(out=xr[iq], in_=rows[bq * 512:(bq + 1) * 512, :].rearrange("(t p) d -> p t d", p=128)), ("xr", iq), writes=[("xr", iq)])

        def xt_transposes(bq):
            iq = bq % 2
            ir = bq % 3
            for t in range(4):
                transpose8(xr[ir][:, t, :], ("xr", ir), XTb[iq][:, :, t * 128:(t + 1) * 128], ("XT", iq), next_bank(allb), evac_eng())

        xr_load(0)
        xr_load(1)
        for blk in range(NBLK):
            i = blk % 2
            for mi, (wb, wkey) in enumerate(((wg_e, "wg_e"), (wu_e, "wu_e"), (wd_e, "wd_e"))):
                S.dma("pool", (lambda mi, wb, i, blk: lambda e: e.indirect_dma_start(out=wb[i].rearrange("p k n -> p (k n)"), out_offset=None, in_=wexp[mi], in_offset=bass.IndirectOffsetOnAxis(ap=idxe_i[:, blk:blk + 1], axis=0)))(mi, wb, i, blk), (wkey, i), writes=[(wkey, i)])
            if blk + 2 < NBLK:
                xr_load(blk + 2)
            XT = XTb[i]
            xtk = ("XT", i)
            if blk == 0:
                xt_transposes(0)
            for fc in range(4):
                bg = next_bank(allb)
                bu = next_bank(allb)
                for kc in range(8):
                    S.op("pe", (lambda fc, kc, bg, i: lambda e: e.matmul(ps[:, bg, :], lhsT=wg_e[i][:, kc, fc * 128:(fc + 1) * 128], rhs=XTb[i][:, kc, :], start=(kc == 0), stop=(kc == 7)))(fc, kc, bg, i), reads=[("wg_e", i), xtk], writes=[PB(bg)])
                for kc in range(8):
                    S.op("pe", (lambda fc, kc, bu, i: lambda e: e.matmul(ps[:, bu, :], lhsT=wu_e[i][:, kc, fc * 128:(fc + 1) * 128], rhs=XTb[i][:, kc, :], start=(kc == 0), stop=(kc == 7)))(fc, kc, bu, i), reads=[("wu_e", i), xtk], writes=[PB(bu)])
                f2 = fc % 2
                S.op("act", (lambda bg, f2: lambda e: e.activation(out=th[f2], in_=ps[:, bg, :], func=AF.Tanh, scale=0.5))(bg, f2), reads=[PB(bg)], writes=[("th", f2), PB(bg)])
                S.op("dve", (lambda bg, f2: lambda e: e.scalar_tensor_tensor(out=ga[f2], in0=th[f2], scalar=1.0, in1=ps[:, bg, :], op0=ALU.add, op1=ALU.mult))(bg, f2), reads=[("th", f2), PB(bg)], writes=[("ga", f2), PB(bg)])
                S.op("dve", (lambda bu, f2, fc: lambda e: e.scalar_tensor_tensor(out=hidT[:, fc, :], in0=ga[f2], scalar=0.5, in1=ps[:, bu, :], op0=ALU.mult, op1=ALU.mult))(bu, f2, fc), reads=[("ga", f2), PB(bu)], writes=["hidT", PB(bu)])
            if blk + 1 < NBLK:
                xt_transposes(blk + 1)
            for t in range(4):
                ybb = yb[t % 2]
                yk = ("yb", t % 2)
                for hf in range(2):
                    bk = next_bank(allb)
                    for fc in range(4):
                        S.op("pe", (lambda t, hf, fc, bk, i: lambda e: e.matmul(ps[:, bk, :], lhsT=hidT[:, fc, t * 128:(t + 1) * 128], rhs=wd_e[i][:, fc, hf * 512:(hf + 1) * 512], start=(fc == 0), stop=(fc == 3)))(t, hf, fc, bk, i), reads=["hidT", ("wd_e", i)], writes=[PB(bk)])
                    copy_op("act" if hf == 0 else "dve", ybb[:, hf * 512:(hf + 1) * 512], ps[:, bk, :], [PB(bk)], [yk, PB(bk)])
                r0 = blk * 512 + t * 128
                S.dma("sp", (lambda r0, ybb: lambda e: e.dma_start(out=yrows[r0:r0 + 128, :], in_=ybb))(r0, ybb), yk, reads=[yk])
        S.barrier()

        A.off = p3_off
        NB4 = 4
        x1b = [A.alloc([128, D], F32) for _ in range(NB4)]
        ya = [A.alloc([128, D], BF16) for _ in range(NB4)]
        yc = [A.alloc([128, D], BF16) for _ in range(NB4)]
        ob = [A.alloc([128, D], F32) for _ in range(NB4)]
        junk2 = A.alloc([128, D], BF16)
        gfin_b = A.alloc([128, D], F32)
        S.dma("sp", lambda e: e.dma_start(out=gfin_b, in_=gfin.partition_broadcast(128)), "gfin_b", writes=["gfin_b"])
        ssf = A.alloc([128, NT], F32)
        def p4_load(T):
            i = T % NB4
            S.dma("sp", lambda e: e.dma_start(out=x1b[i], in_=x1s[T * 128:(T + 1) * 128, :]), ("x1b", i), writes=[("x1b", i)])
            S.dma("pool", lambda e: e.indirect_dma_start(out=ya[i], out_offset=None, in_=yrows, in_offset=bass.IndirectOffsetOnAxis(ap=dhi_i[:, T:T + 1], axis=0)), ("ya", i), writes=[("ya", i)])
            S.dma("pool", lambda e: e.indirect_dma_start(out=yc[i], out_offset=None, in_=yrows, in_offset=bass.IndirectOffsetOnAxis(ap=dlo_i[:, T:T + 1], axis=0)), ("yc", i), writes=[("yc", i)])

        def p4_a(T):
            i = T % NB4
            S.op("dve", lambda e: e.scalar_tensor_tensor(out=x1b[i], in0=ya[i], scalar=whi[:, T:T + 1], in1=x1b[i], op0=ALU.mult, op1=ALU.add), reads=[("ya", i), ("x1b", i)], writes=[("x1b", i)])
            S.op("dve", lambda e: e.scalar_tensor_tensor(out=x1b[i], in0=yc[i], scalar=wlo[:, T:T + 1], in1=x1b[i], op0=ALU.mult, op1=ALU.add), reads=[("yc", i), ("x1b", i)], writes=[("x1b", i)])
            S.op("act", lambda e: e.activation(out=junk2, in_=x1b[i], func=AF.Square, accum_out=ssf[:, T:T + 1]), reads=[("x1b", i)], writes=["junk2", ("ssf", T)])

        def p4_b(T):
            i = T % NB4
            S.op("dve", lambda e: e.tensor_scalar(out=ssf[:, T:T + 1], in0=ssf[:, T:T + 1], scalar1=1.0 / D, scalar2=EPS, op0=ALU.mult, op1=ALU.add), reads=[("ssf", T)], writes=[("ssf", T)])
            S.op("act", lambda e: e.activation(out=ssf[:, T:T + 1], in_=ssf[:, T:T + 1], func=AF.Sqrt), reads=[("ssf", T)], writes=[("ssf", T)])

        def p4_c(T):
            i = T % NB4
            S.op("dve", lambda e: e.reciprocal(out=ssf[:, T:T + 1], in_=ssf[:, T:T + 1]), reads=[("ssf", T)], writes=[("ssf", T)])
            S.op("dve", lambda e: e.scalar_tensor_tensor(out=ob[i], in0=x1b[i], scalar=ssf[:, T:T + 1], in1=gfin_b, op0=ALU.mult, op1=ALU.mult), reads=[("x1b", i), ("ssf", T), "gfin_b"], writes=[("ob", i)])
            S.dma("sp", lambda e: e.dma_start(out=out[T * 128:(T + 1) * 128, :], in_=ob[i]), ("ob", i), reads=[("ob", i)])

        NB4 = 6
        x1b = x1b + [A.alloc([128, D], F32) for _ in range(2)]
        ya = ya + [A.alloc([128, D], BF16) for _ in range(2)]
        yc = yc + [A.alloc([128, D], BF16) for _ in range(2)]
        ob = ob + [A.alloc([128, D], F32) for _ in range(2)]
        for T in range(NB4 - 3):
            p4_load(T)
        for T in range(NT + 2):
            if T + NB4 - 3 < NT:
                p4_load(T + NB4 - 3)
            if T < NT:
                p4_a(T)
            if 0 <= T - 1 < NT:
                p4_b(T - 1)
            if 0 <= T - 2 < NT:
                p4_c(T - 2)
        S.emit()
    return nc


_NC_CACHE = {}


def _prep_inputs(inp):
    f = lambda a: np.ascontiguousarray(np.asarray(a, dtype=np.float32))
    w_in = f(inp["w_in"])[0]
    gates = w_in[:, 2048:].reshape(8, 128, 3, 8, 128).transpose(3, 1, 0, 2, 4).reshape(8, 128, 24, 128)
    pp = f(inp["p_pool"])[0].reshape(2, 128, 8, 128).transpose(2, 1, 0, 3)
    pd = f(inp["p_diff"])[0].reshape(4, 128, 8, 128).transpose(2, 1, 0, 3)
    pm = f(inp["p_mem"])[0].reshape(2, 128, 8, 128).transpose(2, 1, 0, 3)
    w_fc = np.ascontiguousarray(np.concatenate([gates, pp, pd, pm], axis=2)).reshape(8, 128, 32 * 128)
    shared = {
        "gmix": np.ascontiguousarray(f(inp["norm_mix_g"]).reshape(8, 128).T),
        "gmem": f(inp["norm_mem_g"]).reshape(1, D),
        "gffn": f(inp["norm_ffn_g"]).reshape(1, D),
        "gfin": f(inp["final_g"]).reshape(1, D),
        "w_in_a": np.ascontiguousarray(w_in[:, :2048]),
        "w_fc": w_fc,
        "pool_w": f(inp["pool_w"])[0],
        "pscale": np.ascontiguousarray(f(inp["pool_scale"])[0].reshape(2, 128).T),
        "lamv": np.concatenate([f(inp["lam_q1"])[0], f(inp["lam_k1"])[0], f(inp["lam_q2"])[0], f(inp["lam_k2"])[0]]).reshape(1, 256),
        "subg": f(inp["subln_g"]).reshape(1, 128),
        "wkv": f(inp["w_mem_kv"])[0],
        "w_o": f(inp["w_o"])[0],
        "w_r": np.ascontiguousarray(np.concatenate([f(inp["w_router_group"])[0], f(inp["w_router_expert"])[0].reshape(D, 16)], axis=1)),
        "b_r": np.concatenate([f(inp["b_router_group"])[0], f(inp["b_router_expert"])[0].reshape(16)]).reshape(1, 20),
        "w_gate": f(inp["w_expert_gate"])[0].reshape(16 * 1024, 512),
        "w_up": f(inp["w_expert_up"])[0].reshape(16 * 1024, 512),
        "w_down": f(inp["w_expert_down"])[0].reshape(16 * 512, 1024),
    }
    x = f(inp["x"])
    mem = f(inp["mem"])
    maps = []
    for c in range(8):
        m = dict(shared)
        m["x"] = x[4 * c:4 * c + 4].reshape(NTOK, D)
        m["mem"] = mem[4 * c:4 * c + 4].reshape(1024, D)
        maps.append(m)
    return maps


def kernel(**inputs):
    stage = int(inputs.pop("_stage", 9)) if "_stage" in inputs else 9
    if stage not in _NC_CACHE:
        _NC_CACHE[stage] = build_nc(stage)
    nc = _NC_CACHE[stage]
    maps = _prep_inputs(inputs)
    res = run_bass_kernel_spmd(nc, maps, core_ids=list(range(8)))
    outs = [np.asarray(r["out"], dtype=np.float32).reshape(4, 2048, D) for r in res.results]
    return np.concatenate(outs, axis=0)
```

```python
import contextlib
import numpy as np
import concourse.bass as bass
import concourse.mybir as mybir
from concourse.bass_utils import run_bass_kernel_spmd

F32 = mybir.dt.float32
BF16 = mybir.dt.bfloat16
I32 = mybir.dt.int32
AF = mybir.ActivationFunctionType
ALU = mybir.AluOpType
AX = mybir.AxisListType

ENGS = ("pe", "act", "dve", "pool", "sp")


class _Op:
    __slots__ = ("eng", "fn", "deps", "dma_key", "dma_val", "need_inc", "cnt", "dma_waits", "barrier")


class Sched:
    def __init__(self, nc):
        self.nc = nc
        self.ops = []
        self.last_w = {}
        self.readers = {}
        self.dma_cnt = {}
        self.all_dma_keys = []
        self.last_compute = {}

    def _add(self, eng, fn, reads, writes, dma_key=None):
        op = _Op()
        op.eng = eng
        op.fn = fn
        op.dma_key = dma_key
        op.need_inc = False
        op.cnt = 0
        op.barrier = False
        idx = len(self.ops)
        deps = set()
        for r in reads:
            w = self.last_w.get(r)
            if w is not None:
                deps.add(w)
        for r in writes:
            w = self.last_w.get(r)
            if w is not None:
                deps.add(w)
            for rd in self.readers.get(r, ()):
                deps.add(rd)
        op.deps = deps
        op.dma_waits = {}
        for d in deps:
            dop = self.ops[d]
            if dop.dma_key is not None:
                k = dop.dma_key
                op.dma_waits[k] = max(op.dma_waits.get(k, 0), self.dma_cnt[k])
        if dma_key is not None:
            if dma_key not in self.dma_cnt:
                self.dma_cnt[dma_key] = 0
                self.all_dma_keys.append(dma_key)
            self.dma_cnt[dma_key] += 16
            op.dma_val = self.dma_cnt[dma_key]
        else:
            self.last_compute[eng] = idx
        self.ops.append(op)
        for r in reads:
            self.readers.setdefault(r, []).append(idx)
        for r in writes:
            self.last_w[r] = idx
            self.readers[r] = []
        return idx

    def op(self, eng, fn, reads=(), writes=()):
        return self._add(eng, fn, tuple(reads), tuple(writes), None)

    def dma(self, eng, fn, key, reads=(), writes=()):
        return self._add(eng, fn, tuple(reads), tuple(writes), key)

    def barrier(self):
        lc = dict(self.last_compute)
        dw = dict(self.dma_cnt)
        for e in ENGS:
            op = _Op()
            op.eng = e
            op.fn = None
            op.dma_key = None
            op.need_inc = False
            op.cnt = 0
            op.barrier = True
            op.deps = set(lc.values())
            op.dma_waits = dict(dw)
            self.ops.append(op)
        self.last_w = {}
        self.readers = {}

    def emit(self, final_eng="sp"):
        nc = self.nc
        ops = self.ops
        for op in ops:
            for d in op.deps:
                dop = ops[d]
                if dop.dma_key is None:
                    if dop.eng == "pe" and op.eng == "pe" and not op.barrier:
                        continue
                    dop.need_inc = True
        cnt = {e: 0 for e in ENGS}
        for op in ops:
            if op.dma_key is None and op.need_inc:
                cnt[op.eng] += 1
                op.cnt = cnt[op.eng]
        per_eng = {e: [] for e in ENGS}
        for op in ops:
            per_eng[op.eng].append(op)
        with contextlib.ExitStack() as st:
            esem = {e: st.enter_context(nc.semaphore("sem_" + e)) for e in ENGS}
            dsem = {k: st.enter_context(nc.semaphore("dsem_%d" % i)) for i, k in enumerate(self.all_dma_keys)}
            block = st.enter_context(nc.Block())

            def run_engine(ename, engine):
                waited = {}
                for op in per_eng[ename]:
                    need = {}
                    for d in op.deps:
                        dop = ops[d]
                        if dop.dma_key is not None:
                            continue
                        if dop.eng == "pe" and ename == "pe" and not op.barrier:
                            continue
                        s = ("e", dop.eng)
                        need[s] = max(need.get(s, 0), dop.cnt)
                    for k, v in op.dma_waits.items():
                        s = ("d", k)
                        need[s] = max(need.get(s, 0), v)
                    for s, v in need.items():
                        if waited.get(s, 0) >= v:
                            continue
                        waited[s] = v
                        sem = esem[s[1]] if s[0] == "e" else dsem[s[1]]
                        engine.wait_ge(sem, v)
                    if op.fn is None:
                        continue
                    ins = op.fn(engine)
                    if op.dma_key is not None:
                        ins.then_inc(dsem[op.dma_key], 16)
                    elif op.need_inc:
                        ins.then_inc(esem[ename], 1)
                if ename == final_eng:
                    for k in self.all_dma_keys:
                        v = self.dma_cnt[k]
                        if waited.get(("d", k), 0) < v:
                            engine.wait_ge(dsem[k], v)

            block.tensor(lambda e: run_engine("pe", e))
            block.scalar(lambda e: run_engine("act", e))
            block.vector(lambda e: run_engine("dve", e))
            block.gpsimd(lambda e: run_engine("pool", e))
            block.sync(lambda e: run_engine("sp", e))


class Arena:
    def __init__(self, ap):
        self.ap = ap
        self.off = 0
        self.cap = ap.shape[1] * 2

    def alloc(self, shape, dt):
        assert shape[0] == 128
        n = 1
        for s in shape[1:]:
            n *= s
        esz = 2 if dt == BF16 else 4
        nb = n * esz
        start = (self.off + 31) // 32 * 32
        self.off = start + nb
        assert self.off <= self.cap, ("arena overflow", self.off, self.cap)
        a = self.ap[:, start // 2:(start + nb) // 2]
        if dt != BF16:
            a = a.bitcast(dt)
        if len(shape) > 2:
            names = " ".join("d%d" % i for i in range(len(shape) - 1))
            kw = {"d%d" % i: shape[i + 1] for i in range(len(shape) - 1)}
            a = a.rearrange("p (%s) -> p %s" % (names, names), **kw)
        return a


D = 1024
NTOK = 8192
NT = 64
GS = 512
NGRP = 16
NBLK = 48
NROWS = NBLK * 512
EPS = 1e-6
LAMBDA_INIT = 0.8 - 0.6 * 1.0
BIG = 1.0e4


def build_nc(stage=9):
    nc = bass.Bass("TRN2", target_bir_lowering=False)

    def din(name, shape, dt=F32):
        return nc.dram_tensor(name, shape, dt, kind="ExternalInput").ap()

    x = din("x", [NTOK, D])
    mem = din("mem", [1024, D])
    gmix = din("gmix", [128, 8])
    gmem = din("gmem", [1, D])
    gffn = din("gffn", [1, D])
    gfin = din("gfin", [1, D])
    w_in_a = din("w_in_a", [D, 2048])
    w_fc = din("w_fc", [8, 128, 32 * 128])
    pool_w = din("pool_w", [4, 64, 64])
    pscale = din("pscale", [128, 2])
    lamv = din("lamv", [1, 256])
    subg = din("subg", [1, 128])
    wkv = din("wkv", [D, 512])
    w_o = din("w_o", [D, D])
    w_r = din("w_r", [D, 20])
    b_r = din("b_r", [1, 20])
    w_gate = din("w_gate", [16 * 1024, 512])
    w_up = din("w_up", [16 * 1024, 512])
    w_down = din("w_down", [16 * 512, 1024])
    out = nc.dram_tensor("out", [NTOK, D], F32, kind="ExternalOutput").ap()
    x1s = nc.dram_tensor("x1s", [NTOK, D], F32, kind="Internal").ap()
    h2s = nc.dram_tensor("h2s", [NTOK, D], BF16, kind="Internal").ap()
    rows = nc.dram_tensor("rows", [NROWS, D], BF16, kind="Internal").ap()
    yrows = nc.dram_tensor("yrows", [NROWS, D], BF16, kind="Internal").ap()
    mks = nc.dram_tensor("mks", [4, 128, 512], BF16, kind="Internal").ap()
    mvs = nc.dram_tensor("mvs", [4, 128, 528], BF16, kind="Internal").ap()
    wfc_bf = nc.dram_tensor("wfc_bf", [8, 128, 4096], BF16, kind="Internal").ap()
    wexp = [nc.dram_tensor("wexp%d" % i, [16 * 128, 4096], BF16, kind="Internal").ap() for i in range(3)]

    S = Sched(nc)
    with contextlib.ExitStack() as st:
        arena_t = st.enter_context(nc.sbuf_tensor("arena", [128, 106200], BF16))
        ps = st.enter_context(nc.psum_tensor("ps", [128, 8, 512], F32))
        A = Arena(arena_t[:, :])

        def PB(b):
            return ("pb", b)

        bank_rr = [0]

        def next_bank(pool=(0, 1, 2, 3)):
            b = pool[bank_rr[0] % len(pool)]
            bank_rr[0] += 1
            return b

        flip = [0]

        def evac_eng():
            flip[0] ^= 1
            return "act" if flip[0] else "dve"

        def copy_op(eng, out_ap, in_ap, reads, writes):
            if eng == "act":
                S.op("act", lambda e: e.activation(out=out_ap, in_=in_ap, func=AF.Copy), reads, writes)
            elif eng == "dve":
                S.op("dve", lambda e: e.tensor_copy(out_ap, in_ap), reads, writes)
            else:
                S.op("pool", lambda e: e.tensor_copy(out_ap, in_ap), reads, writes)

        ident_bf = A.alloc([128, 128], BF16)
        ident_f = A.alloc([128, 128], F32)
        maskb = A.alloc([128, 128], BF16)
        U_bf = A.alloc([128, 128], BF16)
        ones_bf = A.alloc([128, 128], BF16)
        logits = A.alloc([128, NT, 20], F32)
        dhi_i = A.alloc([128, NT], I32)
        dlo_i = A.alloc([128, NT], I32)
        whi = A.alloc([128, NT], F32)
        wlo = A.alloc([128, NT], F32)
        idxe_i = A.alloc([128, NBLK], I32)
        persist_off = A.off

        S.op("pool", lambda e: e.memset(ident_bf, 1.0), writes=["ident_bf"])
        S.op("pool", lambda e: e.affine_select(out=ident_bf, in_=ident_bf, pattern=[[-1, 128]], compare_op=ALU.is_equal, fill=0.0, base=0, channel_multiplier=1), reads=["ident_bf"], writes=["ident_bf"])
        S.op("pool", lambda e: e.memset(ident_f, 1.0), writes=["ident_f"])
        S.op("pool", lambda e: e.affine_select(out=ident_f, in_=ident_f, pattern=[[-1, 128]], compare_op=ALU.is_equal, fill=0.0, base=0, channel_multiplier=1), reads=["ident_f"], writes=["ident_f"])
        S.op("pool", lambda e: e.memset(maskb, 0.0), writes=["maskb"])
        S.op("pool", lambda e: e.affine_select(out=maskb, in_=maskb, pattern=[[1, 128]], compare_op=ALU.is_ge, fill=-30000.0, base=0, channel_multiplier=-1), reads=["maskb"], writes=["maskb"])
        S.op("pool", lambda e: e.memset(U_bf, 1.0), writes=["U_bf"])
        S.op("pool", lambda e: e.affine_select(out=U_bf, in_=U_bf, pattern=[[1, 128]], compare_op=ALU.is_gt, fill=0.0, base=0, channel_multiplier=-1), reads=["U_bf"], writes=["U_bf"])
        S.op("pool", lambda e: e.memset(ones_bf, 1.0), writes=["ones_bf"])

        w_in_sb = A.alloc([128, 8, 2048], BF16)
        wo_sb = A.alloc([128, 8, D], BF16)
        wr_sb = A.alloc([128, 8, 20], F32)
        br_b = A.alloc([128, 20], F32)
        gmixT = A.alloc([128, 8], F32)
        gffn_b = A.alloc([128, D], F32)
        wbd = A.alloc([128, 2, 128], BF16)
        pscale_s = A.alloc([128, 2], F32)
        subg_b = A.alloc([128, 128], F32)
        lam_b = A.alloc([128, 256], F32)
        lam_t = A.alloc([128, 4], F32)
        invw = A.alloc([128, 2], F32)
        invc0 = A.alloc([128, 2, 16], F32)
        wg_s = [A.alloc([128, 32, 128], BF16) for _ in range(2)]
        x_sb = A.alloc([128, 4, D], F32)
        junk_f = A.alloc([128, D], F32)
        junk_b = A.alloc([128, D], BF16)
        h_tm = [A.alloc([128, D], BF16) for _ in range(2)]
        hT = A.alloc([128, 8, GS], BF16)
        ubuf = A.alloc([128, 2, 528], F32)
        sA = A.alloc([128, 2, 528], F32)
        sB = A.alloc([128, 2, 528], F32)
        sAf = sA.rearrange("p a b -> p (a b)")
        sBf = sB.rearrange("p a b -> p (a b)")
        mtmp = [sAf[:, 0:512], sAf[:, 512:1024], sBf[:, 0:512]]
        mkey = ["sA", "sA", "sB"]
        pooled = A.alloc([128, 2, GS], BF16)
        pool_outT = A.alloc([128, 2, GS], BF16)
        alias_off = A.off
        qT = A.alloc([128, 4, GS], BF16)
        kT = A.alloc([128, 4, 2048], BF16)
        V_aug = A.alloc([128, 16, 4, 130], BF16)
        alias_end = A.off
        mqT = A.alloc([128, 2, GS], BF16)
        mkT_all = A.alloc([128, 1, 2, 256], BF16)
        mv_all = A.alloc([128, 1, 2, 4, 66], BF16)
        pT_all = A.alloc([128, 2, 2, GS], BF16)
        pT = [pT_all[:, 0, :, :], pT_all[:, 1, :, :]]

        def PTK(i):
            return [("pT", i, 0), ("pT", i, 1)]
        h2T_f = pT_all.rearrange("p a b c -> p (a b c)").bitcast(F32).rearrange("p (k t) -> p k t", k=8)
        dall_raw = A.alloc([128, 4 * 4 * 128 * 2], BF16)
        dall = dall_raw.bitcast(F32).rearrange("p (a b d) -> p a b d", a=4, b=4)
        mergedT = dall_raw.rearrange("p (k t) -> p k t", k=8)
        t1buf = A.alloc([128, 128], F32)
        rz = A.alloc([128, 8], F32)
        ss16 = A.alloc([128, 16], F32)
        diff_tm = [A.alloc([128, 4, 128], BF16)] * 2
        diff_outT = A.alloc([128, 4, GS], BF16)
        mem_tm = A.alloc([128, 4, 256], BF16)
        rzm = A.alloc([128, 4, 1], F32)
        mem_outT = A.alloc([128, 2, GS], BF16)
        gate_sb = [A.alloc([128, 3, GS], BF16)] * 2
        ssx = A.alloc([128, 4], F32)
        ss2 = A.alloc([128, 4], F32)
        h2b = [A.alloc([128, D], BF16), junk_b]
        h2f = junk_f
        stg = [A.alloc([128, 2048], BF16) for _ in range(2)]
        p1_end = A.off

        w_in_v = w_in_a.rearrange("(k p) n -> p k n", p=128)
        S.dma("sp", lambda e: e.dma_start(out=wr_sb, in_=w_r.rearrange("(k p) n -> p k n", p=128)), "wr_sb", writes=["wr_sb"])
        S.dma("sp", lambda e: e.dma_start(out=br_b, in_=b_r.partition_broadcast(128)), "br_b", writes=["br_b"])
        S.dma("sp", lambda e: e.dma_start(out=gmixT, in_=gmix), "gmixT", writes=["gmixT"])
        S.dma("sp", lambda e: e.dma_start(out=gffn_b, in_=gffn.partition_broadcast(128)), "gffn_b", writes=["gffn_b"])
        S.dma("sp", lambda e: e.dma_start(out=pscale_s, in_=pscale), "pscale_s", writes=["pscale_s"])
        S.dma("sp", lambda e: e.dma_start(out=subg_b, in_=subg.partition_broadcast(128)), "subg_b", writes=["subg_b"])
        S.dma("sp", lambda e: e.dma_start(out=lam_b, in_=lamv.partition_broadcast(128)), "lam_b", writes=["lam_b"])
        S.op("pool", lambda e: e.memset(wbd, 0.0), writes=["wbd"])
        for gi in range(4):
            c, hb = gi // 2, (gi % 2) * 64
            S.dma("pool", (lambda gi, c, hb: lambda e: e.dma_start(out=wbd[hb:hb + 64, c, hb:hb + 64], in_=pool_w[gi]))(gi, c, hb), "wbd", reads=["wbd"], writes=["wbd"])
        S.op("dve", lambda e: e.tensor_scalar(out=subg_b, in0=subg_b, scalar1=1.0 - LAMBDA_INIT, scalar2=None, op0=ALU.mult), reads=["subg_b"], writes=["subg_b"])
        S.op("dve", lambda e: e.tensor_tensor(out=lam_b[:, 0:64], in0=lam_b[:, 0:64], in1=lam_b[:, 64:128], op=ALU.mult), reads=["lam_b"], writes=["lam_b"])
        S.op("dve", lambda e: e.tensor_tensor(out=lam_b[:, 128:192], in0=lam_b[:, 128:192], in1=lam_b[:, 192:256], op=ALU.mult), reads=["lam_b"], writes=["lam_b"])
        S.op("dve", lambda e: e.tensor_reduce(out=lam_t[:, 0:1], in_=lam_b[:, 0:64], axis=AX.X, op=ALU.add), reads=["lam_b"], writes=["lam_t"])
        S.op("dve", lambda e: e.tensor_reduce(out=lam_t[:, 1:2], in_=lam_b[:, 128:192], axis=AX.X, op=ALU.add), reads=["lam_b"], writes=["lam_t"])
        S.op("act", lambda e: e.activation(out=lam_t[:, 0:2], in_=lam_t[:, 0:2], func=AF.Exp), reads=["lam_t"], writes=["lam_t"])
        S.op("dve", lambda e: e.tensor_tensor(out=lam_t[:, 2:3], in0=lam_t[:, 0:1], in1=lam_t[:, 1:2], op=ALU.subtract), reads=["lam_t"], writes=["lam_t"])
        S.op("dve", lambda e: e.tensor_scalar(out=lam_t[:, 3:4], in0=lam_t[:, 2:3], scalar1=LAMBDA_INIT, scalar2=None, op0=ALU.add), reads=["lam_t"], writes=["lam_t"])
        lam_ap = lam_t[:, 3:4]
        wins = {(0, 0): 2.0, (1, 0): 4.0, (0, 1): 8.0, (1, 1): 16.0}
        for (hh, c), w in wins.items():
            S.op("pool", (lambda hh, c, w: lambda e: e.memset(invw[hh * 64:hh * 64 + 64, c:c + 1], 1.0 / w))(hh, c, w), reads=["invw"], writes=["invw"])
            for j in range(16):
                pass
        for (hh, c), w in wins.items():
            for j in range(16):
                v = 1.0 / min(j + 1.0, w)
                if j + 1 >= w:
                    S.op("pool", (lambda hh, c, j, v: lambda e: e.memset(invc0[hh * 64:hh * 64 + 64, c, j:16], v))(hh, c, j, v), reads=["invc0"], writes=["invc0"])
                    break
                S.op("pool", (lambda hh, c, j, v: lambda e: e.memset(invc0[hh * 64:hh * 64 + 64, c, j:j + 1], v))(hh, c, j, v), reads=["invc0"], writes=["invc0"])
        S.op("pool", lambda e: e.memset(mv_all[:, :, :, :, 64:66], 1.0), writes=["mv_all"])

        def rstd_from_ss(ss_ap, n, key):
            S.op("dve", lambda e: e.tensor_scalar(out=ss_ap, in0=ss_ap, scalar1=1.0 / n, scalar2=EPS, op0=ALU.mult, op1=ALU.add), reads=[key], writes=[key])
            S.op("act", lambda e: e.activation(out=ss_ap, in_=ss_ap, func=AF.Sqrt), reads=[key], writes=[key])
            S.op("dve", lambda e: e.reciprocal(out=ss_ap, in_=ss_ap), reads=[key], writes=[key])

        def sumsq(src_ap, dst_col, src_keys, dst_key):
            S.op("act", lambda e: e.activation(out=junk_b, in_=src_ap, func=AF.Square, accum_out=dst_col), reads=list(src_keys) + [dst_key], writes=["junk_b", dst_key])

        def transpose8(src_tm, src_key, dst_ap, dst_key, bank, eng):
            pbf = ps[:, bank, :].bitcast(BF16)
            for kc in range(8):
                S.op("pe", (lambda kc: lambda e: e.transpose(pbf[:, kc * 128:(kc + 1) * 128], src_tm[:, kc * 128:(kc + 1) * 128], ident_bf))(kc), reads=[src_key, "ident_bf"], writes=[PB(bank)])
            if eng == "gain":
                S.op("dve", lambda e: e.tensor_tensor(out=dst_ap, in0=pbf.rearrange("p (k t) -> p k t", k=8), in1=gmixT.unsqueeze(2).to_broadcast([128, 8, 128]), op=ALU.mult), reads=[PB(bank), "gmixT"], writes=[dst_key, PB(bank)])
            else:
                copy_op(eng, dst_ap, pbf.rearrange("p (k t) -> p k t", k=8), [PB(bank)], [dst_key, PB(bank)])

        def conv_chunks(ex):
            ch = []
            for mi, wsrc in ((0, w_gate), (1, w_up)):
                for j in range(2):
                    src = wsrc[ex * 1024 + j * 512: ex * 1024 + (j + 1) * 512, :].rearrange("(k p) n -> p k n", p=128)
                    dst = wexp[mi][ex * 128:(ex + 1) * 128, j * 2048:(j + 1) * 2048].rearrange("p (k n) -> p k n", k=4)
                    ch.append((src, dst, 4))
            for j in range(2):
                src = w_down[ex * 512 + j * 256: ex * 512 + (j + 1) * 256, :].rearrange("(k p) n -> p k n", p=128)
                dst = wexp[2][ex * 128:(ex + 1) * 128, j * 2048:(j + 1) * 2048].rearrange("p (k n) -> p k n", k=2)
                ch.append((src, dst, 2))
            return ch

        def conv_step(ch, k):
            n = len(ch)
            if 1 <= k <= n:
                src, dst, kk = ch[k - 1]
                sb_ = stg[(k - 1) % 2]
                S.dma("pool", (lambda dst, sb_, kk: lambda e: e.dma_start(out=dst, in_=sb_.rearrange("p (k n) -> p k n", k=kk)))(dst, sb_, kk), "wexpst", reads=[("stg", (k - 1) % 2)])
            if k < n:
                src, dst, kk = ch[k]
                sb_ = stg[k % 2]
                S.dma("pool", (lambda src, sb_, kk: lambda e: e.dma_start(out=sb_.rearrange("p (k n) -> p k n", k=kk), in_=src))(src, sb_, kk), ("stg", k % 2), writes=[("stg", k % 2)])

        _save_off = A.off
        A.off = alias_off
        wkv_sb = A.alloc([128, 8, 512], BF16)
        gmem_b = A.alloc([128, D], F32)
        mn_tm = A.alloc([128, D], BF16)
        mnT = A.alloc([128, 8, 256], BF16)
        ssm = A.alloc([128, 2], F32)
        mem_sb = A.alloc([128, 2, D], F32)
        assert A.off <= alias_end, (A.off, alias_end)
        A.off = _save_off

        def mem_prologue(b):
            if b == 0:
                S.dma("pool", lambda e: e.dma_start(out=wkv_sb, in_=wkv.rearrange("(k p) n -> p k n", p=128)), "wkv_sb", writes=["wkv_sb"])
                S.dma("sp", lambda e: e.dma_start(out=gmem_b, in_=gmem.partition_broadcast(128)), "gmem_b", writes=["gmem_b"])
            for mt in range(2):
                r0 = b * 256 + mt * 128
                S.dma("sp", (lambda mt, r0: lambda e: e.dma_start(out=mem_sb[:, mt, :], in_=mem[r0:r0 + 128, :]))(mt, r0), ("mem_sb", mt), writes=[("mem_sb", mt)])
                sumsq(mem_sb[:, mt, :], ssm[:, mt:mt + 1], [("mem_sb", mt)], "ssm")
            rstd_from_ss(ssm, float(D), "ssm")
            for mt in range(2):
                S.op("dve", (lambda mt: lambda e: e.scalar_tensor_tensor(out=mn_tm, in0=mem_sb[:, mt, :], scalar=ssm[:, mt:mt + 1], in1=gmem_b, op0=ALU.mult, op1=ALU.mult))(mt), reads=[("mem_sb", mt), "ssm", "gmem_b"], writes=["mn_tm"])
                transpose8(mn_tm, "mn_tm", mnT[:, :, mt * 128:(mt + 1) * 128], "mnT", next_bank(), evac_eng())
            for j in range(2):
                bk = next_bank()
                for kc in range(8):
                    S.op("pe", (lambda j, kc, bk: lambda e: e.matmul(ps[:, bk, 0:256], lhsT=wkv_sb[:, kc, j * 128:(j + 1) * 128], rhs=mnT[:, kc, :], start=(kc == 0), stop=(kc == 7)))(j, kc, bk), reads=["wkv_sb", "mnT"], writes=[PB(bk)])
                copy_op(evac_eng(), mkT_all[:, 0, j, :], ps[:, bk, 0:256], [PB(bk)], ["mkT_all", PB(bk)])
            for mt in range(2):
                bk = next_bank()
                for kc in range(8):
                    S.op("pe", (lambda mt, kc, bk: lambda e: e.matmul(ps[:, bk, 0:256], lhsT=mnT[:, kc, mt * 128:(mt + 1) * 128], rhs=wkv_sb[:, kc, 256:512], start=(kc == 0), stop=(kc == 7)))(mt, kc, bk), reads=["wkv_sb", "mnT"], writes=[PB(bk)])
                copy_op(evac_eng(), mv_all[:, 0, mt, :, 0:64], ps[:, bk, 0:256].rearrange("p (h d) -> p h d", h=4), [PB(bk)], ["mv_all", PB(bk)])
            S.dma("sp", lambda e: e.dma_start(out=mks[b], in_=mkT_all.rearrange("p a j m -> p (a j m)")), "mkst", reads=["mkT_all"], writes=["mks"])
            S.dma("sp", lambda e: e.dma_start(out=mvs[b], in_=mv_all.rearrange("p a m h d -> p (a m h d)")), "mvst", reads=["mv_all"], writes=["mvs"])

        def mem_reload(b):
            S.dma("sp", lambda e: e.dma_start(out=mkT_all.rearrange("p a j m -> p (a j m)"), in_=mks[b]), "mkT_ld", reads=["mks"], writes=["mkT_all"])
            S.dma("sp", lambda e: e.dma_start(out=mv_all.rearrange("p a m h d -> p (a m h d)"), in_=mvs[b]), "mv_ld", reads=["mvs"], writes=["mv_all"])

        allb = tuple(range(8))
        xh = [stg[t // 2][:, (t % 2) * 1024:(t % 2 + 1) * 1024] for t in range(4)]

        def head_load(g):
            tok0 = g * GS
            for t in range(4):
                S.dma("pool", (lambda t: lambda e: e.dma_start(out=xh[t], in_=x[tok0 + t * 128: tok0 + (t + 1) * 128, :]))(t), ("stg", t // 2), writes=[("stg", t // 2)])

        def head_stats(g):
            for t in range(4):
                sumsq(xh[t], ssx[:, t:t + 1], [("stg", t // 2)], "ssx")
            rstd_from_ss(ssx, float(D), "ssx")

        def head_T(g, t):
            hb = h_tm[t % 2]
            hk = ("h_tm", t % 2)
            S.op("dve", lambda e: e.tensor_scalar(out=hb, in0=xh[t], scalar1=ssx[:, t:t + 1], scalar2=None, op0=ALU.mult), reads=[("stg", t // 2), "ssx"], writes=[hk])
            transpose8(hb, hk, hT[:, :, t * 128:(t + 1) * 128], "hT", next_bank(), "gain")

        def x_res_load(g):
            tok0 = g * GS
            for t in range(4):
                S.dma("sp", (lambda t: lambda e: e.dma_start(out=x_sb[:, t, :], in_=x[tok0 + t * 128: tok0 + (t + 1) * 128, :]))(t), ("x_sb", t), writes=[("x_sb", t)])

        def proj_part(g):
            b, gs, tok0 = g // 4, g % 4, g * GS
            if gs == 0:
                S.op("pool", lambda e: e.memset(ubuf[:, :, 0:16], 0.0), reads=["ubuf"], writes=["ubuf"])
            for c in [0, 1, 2, 3, 4, 5, 6, 7, 8, 9, 14, 15]:
                bk = next_bank()
                for kc in range(8):
                    S.op("pe", (lambda c, kc, bk: lambda e: e.matmul(ps[:, bk, :], lhsT=w_in_sb[:, kc, c * 128:(c + 1) * 128], rhs=hT[:, kc, :], start=(kc == 0), stop=(kc == 7)))(c, kc, bk), reads=["w_in_sb", "hT"], writes=[PB(bk)])
                if c < 2:
                    copy_op(evac_eng(), ubuf[:, c, 16:528], ps[:, bk, :], [PB(bk)], ["ubuf", PB(bk)])
                elif c < 6:
                    copy_op(evac_eng(), qT[:, c - 2, :], ps[:, bk, :], [PB(bk)], ["qT", PB(bk)])
                elif c < 10:
                    copy_op(evac_eng(), kT[:, c - 6, gs * GS:(gs + 1) * GS], ps[:, bk, :], [PB(bk)], ["kT", PB(bk)])
                else:
                    copy_op(evac_eng(), mqT[:, c - 14, :], ps[:, bk, :], [PB(bk)], ["mqT", PB(bk)])
            for t in range(4):
                bk = next_bank()
                for kc in range(8):
                    S.op("pe", (lambda t, kc, bk: lambda e: e.matmul(ps[:, bk, :], lhsT=hT[:, kc, t * 128:(t + 1) * 128], rhs=w_in_sb[:, kc, 1280:1792], start=(kc == 0), stop=(kc == 7)))(t, kc, bk), reads=["w_in_sb", "hT"], writes=[PB(bk)])
                copy_op(evac_eng(), V_aug[:, gs * 4 + t, :, 0:128], ps[:, bk, :].rearrange("p (h d) -> p h d", h=4), [PB(bk)], ["V_aug", PB(bk)])

            if g == 0:
                wo_v = w_o.rearrange("(k p) n -> p k n", p=128)
                for kc in range(0, 8, 4):
                    S.dma("pool", (lambda kc: lambda e: e.dma_start(out=wo_sb[:, kc:kc + 4, :], in_=wo_v[:, kc:kc + 4, :]))(kc), "wo_sb", writes=["wo_sb"])
                for fc in range(8):
                    wgs = wg_s[fc % 2]
                    wk = ("wg_s", fc % 2)
                    S.dma("pool", (lambda fc, wgs: lambda e: e.dma_start(out=wgs, in_=w_fc[fc].rearrange("p (k c) -> p k c", k=32)))(fc, wgs), ("wg_conv", fc % 2), writes=[wk])
                    S.dma("sp", (lambda fc, wgs: lambda e: e.dma_start(out=wfc_bf[fc].rearrange("p (k c) -> p k c", k=32), in_=wgs))(fc, wgs), "wfcst", reads=[wk], writes=["wfc_bf"])


        def mid_part(g):
            b, gs, tok0 = g // 4, g % 4, g * GS
            S.op("pool", lambda e: e.tensor_tensor(out=sA[:, :, 1:528], in0=ubuf[:, :, 1:528], in1=ubuf[:, :, 0:527], op=ALU.add), reads=["ubuf"], writes=["sA"])
            S.op("pool", lambda e: e.tensor_tensor(out=sB[:, :, 3:528], in0=sA[:, :, 3:528], in1=sA[:, :, 1:526], op=ALU.add), reads=["sA"], writes=["sB"])

            def pool_fin(src, hh, c):
                p0 = hh * 64
                sl = src[p0:p0 + 64, c, 16:528]
                if gs == 0:
                    S.op("pool", lambda e: e.tensor_tensor(out=sA[p0:p0 + 64, c, 0:16], in0=src[p0:p0 + 64, c, 16:32], in1=invc0[p0:p0 + 64, c, :], op=ALU.mult), reads=["sA", "sB", "invc0"], writes=["sA"])
                S.op("pool", lambda e: e.tensor_scalar(out=sl, in0=sl, scalar1=invw[p0:p0 + 64, c:c + 1], scalar2=None, op0=ALU.mult), reads=["sA", "sB", "invw"], writes=["sA", "sB"])
                S.op("pool", lambda e: e.tensor_tensor(out=pooled[p0:p0 + 64, c, :], in0=sl, in1=ubuf[p0:p0 + 64, c, 16:528], op=ALU.subtract), reads=["sA", "sB", "ubuf"], writes=["pooled"])
                if gs == 0:
                    S.op("pool", lambda e: e.tensor_tensor(out=pooled[p0:p0 + 64, c, 0:16], in0=sA[p0:p0 + 64, c, 0:16], in1=ubuf[p0:p0 + 64, c, 16:32], op=ALU.subtract), reads=["sA", "ubuf"], writes=["pooled"])

            pool_fin(sA, 0, 0)
            pool_fin(sB, 1, 0)
            S.op("pool", lambda e: e.tensor_tensor(out=sA[:, 1, 7:528], in0=sB[:, 1, 7:528], in1=sB[:, 1, 3:524], op=ALU.add), reads=["sB", "sA"], writes=["sA"])
            pool_fin(sA, 0, 1)
            S.op("pool", lambda e: e.tensor_tensor(out=sB[64:128, 1, 15:528], in0=sA[64:128, 1, 15:528], in1=sA[64:128, 1, 7:520], op=ALU.add), reads=["sA", "sB"], writes=["sB"])
            pool_fin(sB, 1, 1)
            if gs < 3:
                S.op("pool", lambda e: e.tensor_copy(sA[:, :, 0:16], ubuf[:, :, 512:528]), reads=["ubuf", "sA"], writes=["sA"])
                S.op("pool", lambda e: e.tensor_copy(ubuf[:, :, 0:16], sA[:, :, 0:16]), reads=["sA", "ubuf"], writes=["ubuf"])
            nkc = 4 * gs + 4
            cchunks = []
            if gs == 1:
                cchunks = conv_chunks(4 * b)
            elif gs == 2:
                cchunks = conv_chunks(4 * b + 1)
            elif gs == 3:
                cchunks = conv_chunks(4 * b + 2) + conv_chunks(4 * b + 3)
            nsteps = len(cchunks) + 1 if cchunks else 0
            per_head = (nsteps + 3) // 4
            for h in range(4):
                started = set()

                def acc_ap(m, qt):
                    a = m * 4 + qt
                    return 4 + a // 3, (a % 3) * 130

                def scores(kc, i):
                    j = kc - 4 * gs
                    q0 = max(j, 0) * 128
                    for m in range(2):
                        bk = 2 * i + m
                        S.op("pe", (lambda m, bk, kc, q0, j, h: lambda e: e.matmul(ps[:, bk, q0:GS], lhsT=kT[m * 64:(m + 1) * 64, h, kc * 128:(kc + 1) * 128], rhs=qT[m * 64:(m + 1) * 64, h, q0:GS], start=True, stop=(j < 0)))(m, bk, kc, q0, j, h), reads=["kT", "qT"], writes=[PB(bk)])
                        if j >= 0:
                            S.op("pe", (lambda bk, q0: lambda e: e.matmul(ps[:, bk, q0:q0 + 128], lhsT=ident_bf, rhs=maskb, start=False, stop=True))(bk, q0), reads=["ident_bf", "maskb"], writes=[PB(bk)])
                    segs = [(q0, 256), (256, GS)] if q0 < 256 else [(q0, GS)]
                    for (qa_, qb_) in segs:
                        S.op("act", (lambda i, qa_, qb_: lambda e: e.activation(out=pT[i][:, :, qa_:qb_], in_=ps[:, 2 * i:2 * i + 2, qa_:qb_], func=AF.Exp, scale=0.125))(i, qa_, qb_), reads=[PB(2 * i), PB(2 * i + 1)], writes=[("pT", i, 0 if qa_ < 256 else 1)])

                def pv(kc, i):
                    j = kc - 4 * gs
                    for qt in range(max(j, 0), 4):
                        for m in range(2):
                            bk, off = acc_ap(m, qt)
                            first = bk not in started
                            started.add(bk)
                            last = (kc == 4 * gs + qt)
                            S.op("pe", (lambda m, qt, bk, off, first, last, kc, i, h: lambda e: e.matmul(ps[:, bk, off:off + 129], lhsT=pT[i][:, m, qt * 128:(qt + 1) * 128], rhs=V_aug[:, kc, h, 0:129], start=first, stop=last, skip_group_check=True))(m, qt, bk, off, first, last, kc, i, h), reads=[("pT", i, qt // 2), "V_aug"], writes=[PB(bk)])

                scores(0, 0)
                for kc in range(nkc):
                    if kc + 1 < nkc:
                        scores(kc + 1, (kc + 1) % 2)
                    pv(kc, kc % 2)
                for k in range(h * per_head, min((h + 1) * per_head, nsteps)):
                    conv_step(cchunks, k)
                for m in range(2):
                    for qt in range(4):
                        bk, off = acc_ap(m, qt)
                        a = m * 4 + qt
                        S.op("dve", (lambda bk, off, a: lambda e: e.reciprocal(out=rz[:, a:a + 1], in_=ps[:, bk, off + 128:off + 129]))(bk, off, a), reads=[PB(bk)], writes=["rz", PB(bk)])
                for qt in range(4):
                    bk1, off1 = acc_ap(1, qt)
                    bk0, off0 = acc_ap(0, qt)
                    S.op("dve", (lambda bk1, off1, qt: lambda e: e.tensor_scalar(out=t1buf, in0=ps[:, bk1, off1:off1 + 128], scalar1=rz[:, 4 + qt:5 + qt], scalar2=lam_ap, op0=ALU.mult, op1=ALU.mult))(bk1, off1, qt), reads=[PB(bk1), "rz", "lam_t"], writes=["t1buf", PB(bk1)])
                    S.op("dve", (lambda bk0, off0, qt, h: lambda e: e.scalar_tensor_tensor(out=dall[:, qt, h, :], in0=ps[:, bk0, off0:off0 + 128], scalar=rz[:, qt:qt + 1], in1=t1buf, op0=ALU.mult, op1=ALU.subtract))(bk0, off0, qt, h), reads=[PB(bk0), "rz", "t1buf"], writes=["dall", PB(bk0)])
            for h in range(4):
                j, pb_ = h // 2, (h % 2) * 64
                i = h % 2
                for mc in range(2):
                    bk = mc
                    S.op("pe", (lambda mc, bk, pb_, j: lambda e: e.matmul(ps[:, bk, :], lhsT=mkT_all[pb_:pb_ + 64, 0, j, mc * 128:(mc + 1) * 128], rhs=mqT[pb_:pb_ + 64, j, :], start=True, stop=True))(mc, bk, pb_, j), reads=["mkT_all", "mqT"], writes=[PB(bk)])
                S.op("act", (lambda i: lambda e: e.activation(out=pT[i], in_=ps[:, 0:2, :], func=AF.Exp, scale=0.125))(i), reads=[PB(0), PB(1)], writes=[*PTK(i), PB(0), PB(1)])
                bka = 2 + h
                for qt in range(4):
                    for mc in range(2):
                        S.op("pe", (lambda qt, mc, i, bka, h: lambda e: e.matmul(ps[:, bka, qt * 66:qt * 66 + 65], lhsT=pT[i][:, mc, qt * 128:(qt + 1) * 128], rhs=mv_all[:, 0, mc, h, 0:65], start=(qt == 0 and mc == 0), stop=(mc == 1), skip_group_check=True))(qt, mc, i, bka, h), reads=[*PTK(i), "mv_all"], writes=[PB(bka)])
            dflat = dall.rearrange("p a b d -> p (a b d)")
            for hf in range(2):
                S.op("dve", (lambda hf: lambda e: e.tensor_tensor(out=junk_f, in0=dflat[:, hf * 1024:(hf + 1) * 1024], in1=dflat[:, hf * 1024:(hf + 1) * 1024], op=ALU.mult))(hf), reads=["dall"], writes=["junk_f"])
                S.op("dve", (lambda hf: lambda e: e.tensor_reduce(out=ss16[:, hf * 8:(hf + 1) * 8], in_=junk_f.rearrange("p (a d) -> p a d", d=128), axis=AX.X, op=ALU.add))(hf), reads=["junk_f"], writes=["ss16"])
            rstd_from_ss(ss16, 128.0, "ss16")
            for qt in range(4):
                dt_ = diff_tm[qt % 2]
                dk = ("diff_tm", 0)
                S.op("dve", (lambda qt: lambda e: e.tensor_tensor(out=dall[:, qt, :, :], in0=dall[:, qt, :, :], in1=ss16[:, qt * 4:(qt + 1) * 4].unsqueeze(2).to_broadcast([128, 4, 128]), op=ALU.mult))(qt), reads=["dall", "ss16"], writes=["dall"])
                S.op("dve", (lambda qt, dt_: lambda e: e.tensor_tensor(out=dt_, in0=dall[:, qt, :, :], in1=subg_b.unsqueeze(1).to_broadcast([128, 4, 128]), op=ALU.mult))(qt, dt_), reads=["dall", "subg_b"], writes=[dk])
                pbf = ps[:, 7, :].bitcast(BF16)
                for hh in range(4):
                    S.op("pe", (lambda hh, dt_: lambda e: e.transpose(pbf[:, hh * 128:(hh + 1) * 128], dt_[:, hh, :], ident_bf))(hh, dt_), reads=[dk, "ident_bf"], writes=[PB(7)])
                copy_op(evac_eng(), diff_outT[:, :, qt * 128:(qt + 1) * 128], pbf[:, 0:512].rearrange("p (k t) -> p k t", k=4), [PB(7)], ["diff_outT", PB(7)])

            for h in range(4):
                bka = 2 + h
                psv = ps[:, bka, 0:264].rearrange("p (q c) -> p q c", c=66)
                S.op("dve", (lambda psv: lambda e: e.reciprocal(out=rzm, in_=psv[:, :, 64:65]))(psv), reads=[PB(bka)], writes=["rzm", PB(bka)])
                S.op("dve", (lambda psv, h: lambda e: e.tensor_tensor(out=mem_tm[:, :, h * 64:(h + 1) * 64], in0=psv[:, :, 0:64], in1=rzm.to_broadcast([128, 4, 64]), op=ALU.mult))(psv, h), reads=[PB(bka), "rzm"], writes=["mem_tm", PB(bka)])
            for qt in range(4):
                pbf = ps[:, 7, :].bitcast(BF16)
                for cc in range(2):
                    S.op("pe", (lambda qt, cc: lambda e: e.transpose(pbf[:, cc * 128:(cc + 1) * 128], mem_tm[:, qt, cc * 128:(cc + 1) * 128], ident_bf))(qt, cc), reads=["mem_tm", "ident_bf"], writes=[PB(7)])
                copy_op(evac_eng(), mem_outT[:, :, qt * 128:(qt + 1) * 128], pbf[:, 0:256].rearrange("p (k t) -> p k t", k=2), [PB(7)], ["mem_outT", PB(7)])

            for c in range(2):
                bk = next_bank()
                S.op("pe", (lambda c, bk: lambda e: e.matmul(ps[:, bk, :], lhsT=wbd[:, c, :], rhs=pooled[:, c, :], start=True, stop=True))(c, bk), reads=["wbd", "pooled"], writes=[PB(bk)])
                S.op("act", (lambda c, bk: lambda e: e.activation(out=pool_outT[:, c, :], in_=ps[:, bk, :], func=AF.Copy, scale=pscale_s[:, c:c + 1]))(c, bk), reads=[PB(bk), "pscale_s"], writes=["pool_outT", PB(bk)])


        def merge_part(g, hook):
            b, gs, tok0 = g // 4, g % 4, g * GS
            allb = tuple(range(8))
            for fc in range(8):
                wgs = wg_s[fc % 2]
                wk = ("wg_s", fc % 2)
                S.dma("sp", (lambda fc, wgs: lambda e: e.dma_start(out=wgs, in_=wfc_bf[fc].rearrange("p (k c) -> p k c", k=32)))(fc, wgs), wk, reads=["wfc_bf"], writes=[wk])
                gsb = gate_sb[fc % 2]
                gk = ("gate_sb", 0)
                for br in range(3):
                    bk = next_bank(allb)
                    for kc in range(8):
                        S.op("pe", (lambda br, kc, bk, wgs: lambda e: e.matmul(ps[:, bk, :], lhsT=wgs[:, kc * 3 + br, :], rhs=hT[:, kc, :], start=(kc == 0), stop=(kc == 7)))(br, kc, bk, wgs), reads=[wk, "hT"], writes=[PB(bk)])
                    S.op("act", (lambda br, bk, gsb: lambda e: e.activation(out=gsb[:, br, :], in_=ps[:, bk, :], func=AF.Tanh, scale=0.5))(br, bk, gsb), reads=[PB(bk)], writes=[gk, PB(bk)])
                srcs = [(24, pool_outT, "pool_outT", 2), (26, diff_outT, "diff_outT", 4), (30, mem_outT, "mem_outT", 2)]
                for br, (wbase, act_, akey, nk) in enumerate(srcs):
                    bk = next_bank(allb)
                    for kc in range(nk):
                        S.op("pe", (lambda kc, bk, wbase, act_, nk, wgs: lambda e: e.matmul(ps[:, bk, :], lhsT=wgs[:, wbase + kc, :], rhs=act_[:, kc, :], start=(kc == 0), stop=(kc == nk - 1)))(kc, bk, wbase, act_, nk, wgs), reads=[wk, akey], writes=[PB(bk)])
                    S.op("dve", (lambda br, bk, gsb: lambda e: e.scalar_tensor_tensor(out=mtmp[br], in0=gsb[:, br, :], scalar=1.0, in1=ps[:, bk, :], op0=ALU.add, op1=ALU.mult))(br, bk, gsb), reads=[gk, PB(bk)], writes=[mkey[br], PB(bk)])
                S.op("pool", lambda e: e.tensor_tensor(out=mtmp[0], in0=mtmp[0], in1=mtmp[1], op=ALU.add), reads=["sA"], writes=["sA"])
                S.op("pool", (lambda fc: lambda e: e.tensor_tensor(out=mergedT[:, fc, :], in0=mtmp[0], in1=mtmp[2], op=ALU.add))(fc), reads=["sA", "sB"], writes=["dall"])
                if hook is not None:
                    hook(fc)


        def wo_part(g):
            b, gs, tok0 = g // 4, g % 4, g * GS
            for t in range(4):
                for hf in range(2):
                    bk = next_bank(allb)
                    for kc in range(8):
                        S.op("pe", (lambda t, hf, kc, bk: lambda e: e.matmul(ps[:, bk, :], lhsT=mergedT[:, kc, t * 128:(t + 1) * 128], rhs=wo_sb[:, kc, hf * 512:(hf + 1) * 512], start=(kc == 0), stop=(kc == 7)))(t, hf, kc, bk), reads=["dall", "wo_sb"], writes=[PB(bk)])
                    S.op("dve", (lambda t, hf, bk: lambda e: e.scalar_tensor_tensor(out=x_sb[:, t, hf * 512:(hf + 1) * 512], in0=ps[:, bk, :], scalar=0.5, in1=x_sb[:, t, hf * 512:(hf + 1) * 512], op0=ALU.mult, op1=ALU.add))(t, hf, bk), reads=[PB(bk), ("x_sb", t)], writes=[("x_sb", t), PB(bk)])
                S.dma("sp", (lambda t, tok0: lambda e: e.dma_start(out=x1s[tok0 + t * 128: tok0 + (t + 1) * 128, :], in_=x_sb[:, t, :]))(t, tok0), ("x_sb", t), reads=[("x_sb", t)])
                sumsq(x_sb[:, t, :], ss2[:, t:t + 1], [("x_sb", t)], "ss2")
            if stage != 1:
                rstd_from_ss(ss2, float(D), "ss2")

        def tail_part(g):
            b, gs, tok0 = g // 4, g % 4, g * GS
            for t in range(4):
                T = g * 4 + t
                hb2 = h2b[t % 2]
                hk2 = ("h2b", 0) if t % 2 == 0 else "junk_b"
                S.op("dve", (lambda t: lambda e: e.scalar_tensor_tensor(out=h2f, in0=x_sb[:, t, :], scalar=ss2[:, t:t + 1], in1=gffn_b, op0=ALU.mult, op1=ALU.mult))(t), reads=[("x_sb", t), "ss2", "gffn_b"], writes=["junk_f"])
                S.op("act", (lambda hb2: lambda e: e.activation(out=hb2, in_=h2f, func=AF.Copy))(hb2), reads=["junk_f"], writes=[hk2])
                S.dma("sp", (lambda T, hb2: lambda e: e.dma_start(out=h2s[T * 128:(T + 1) * 128, :], in_=hb2))(T, hb2), hk2, reads=[hk2])
                for half in range(2):
                    bk = next_bank(allb)
                    for kk in range(4):
                        kc = half * 4 + kk
                        S.op("pe", (lambda kc, kk, bk: lambda e: e.transpose(ps[:, bk, kk * 128:(kk + 1) * 128], h2f[:, kc * 128:(kc + 1) * 128], ident_f))(kc, kk, bk), reads=["junk_f", "ident_f"], writes=[PB(bk)])
                    copy_op(evac_eng(), h2T_f[:, half * 4:(half + 1) * 4, :], ps[:, bk, :].rearrange("p (k t) -> p k t", k=4), [PB(bk)], [*PTK(0), *PTK(1), PB(bk)])
                bk = next_bank(allb)
                for kc in range(8):
                    S.op("pe", (lambda kc, bk: lambda e: e.matmul(ps[:, bk, 0:20], lhsT=h2T_f[:, kc, :], rhs=wr_sb[:, kc, :], start=(kc == 0), stop=(kc == 7)))(kc, bk), reads=[*PTK(0), *PTK(1), "wr_sb"], writes=[PB(bk)])
                S.op("dve", (lambda T, bk: lambda e: e.tensor_tensor(out=logits[:, T, :], in0=ps[:, bk, 0:20], in1=br_b, op=ALU.add))(T, bk), reads=[PB(bk), "br_b"], writes=["logits", PB(bk)])


        head_load(0)
        head_stats(0)
        for t in range(4):
            head_T(0, t)
        for g in range(NGRP):
            b, gs = g // 4, g % 4
            if g == 0:
                S.barrier()
                for bb in range(4):
                    mem_prologue(bb)
                    if bb == 0:
                        for kc in range(8):
                            S.dma("pool", (lambda kc: lambda e: e.dma_start(out=w_in_sb[:, kc, :], in_=w_in_v[:, kc, :]))(kc), "w_in_sb", writes=["w_in_sb"])
                S.barrier()
                S.op("pool", lambda e: e.memset(V_aug[:, :, :, 128:130], 1.0), writes=["V_aug"])
            if gs == 0:
                mem_reload(b)
            proj_part(g)
            if g > 0 and stage != 1:
                tail_part(g - 1)
            x_res_load(g)
            mid_part(g)
            nxt = g + 1 < NGRP
            if nxt:
                head_load(g + 1)

            def hook(fc, g=g):
                if fc == 3:
                    head_stats(g + 1)
            merge_part(g, hook if nxt else None)
            if nxt:
                for t in range(4):
                    head_T(g + 1, t)
            wo_part(g)
        if stage != 1:
            tail_part(NGRP - 1)

        S.barrier()
        if stage == 1:
            A.off = persist_off
            cbuf = [A.alloc([128, D], F32) for _ in range(2)]
            for T in range(NT):
                cb = cbuf[T % 2]
                ck = ("cbuf", T % 2)
                S.dma("sp", (lambda T, cb: lambda e: e.dma_start(out=cb, in_=x1s[T * 128:(T + 1) * 128, :]))(T, cb), ck, writes=[ck])
                S.dma("sp", (lambda T, cb: lambda e: e.dma_start(out=out[T * 128:(T + 1) * 128, :], in_=cb))(T, cb), ("co", T % 2), reads=[ck])
            S.emit()
            return nc

        A.off = persist_off
        Lg = logits[:, :, 0:4]
        Le = logits[:, :, 4:20]
        mg = A.alloc([128, NT], F32)
        G = A.alloc([128, NT, 4], F32)
        eg = A.alloc([128, NT, 4], F32)
        gp = A.alloc([128, NT], F32)
        Lp = A.alloc([128, NT, 16], F32)
        m1 = A.alloc([128, NT], F32)
        m2 = A.alloc([128, NT], F32)
        eq1 = A.alloc([128, NT, 16], F32)
        L2 = A.alloc([128, NT, 16], F32)
        Ssel = A.alloc([128, NT, 16], F32)
        ew = A.alloc([128, NT, 16], F32)
        Wt = A.alloc([128, NT, 16], F32)
        S_bf = A.alloc([128, NT, 16], BF16)
        totS = A.alloc([128, NT, 16], F32)
        totB = A.alloc([128, NT, 16], F32)
        rankS = A.alloc([128, NT, 16], F32)
        cntv = A.alloc([128, 16], F32)
        thr_i = A.alloc([128, 16, 32], I32)
        thr = A.alloc([128, 16, 32], F32)
        cmp = A.alloc([128, 16, 32], F32)
        padded = A.alloc([128, 16], F32)
        pst = A.alloc([128, 16], F32)
        pend = A.alloc([128, 16], F32)
        bst_i = A.alloc([128, NBLK], I32)
        bst = A.alloc([128, NBLK], F32)
        cmpb = A.alloc([128, NBLK, 16], F32)
        be = A.alloc([128, NBLK], F32)
        pk_i = A.alloc([128, 8], I32)
        pk = A.alloc([128, 8], F32)
        idxf = A.alloc([128, NBLK], F32)
        dhi = A.alloc([128, NT], F32)
        dlo = A.alloc([128, NT], F32)
        R = ["rt"]

        def dv(fn, extra_r=(), extra_w=()):
            S.op("dve", fn, reads=R + list(extra_r), writes=R + list(extra_w))

        def bc3(ap2, n):
            return ap2.unsqueeze(2).to_broadcast([128, NT, n])

        dv(lambda e: e.tensor_reduce(out=mg, in_=Lg, axis=AX.X, op=ALU.max), extra_r=["logits"])
        dv(lambda e: e.tensor_tensor(out=G, in0=Lg, in1=bc3(mg, 4), op=ALU.is_ge), extra_r=["logits"])
        dv(lambda e: e.tensor_tensor(out=eg, in0=Lg, in1=bc3(mg, 4), op=ALU.subtract), extra_r=["logits"])
        S.op("act", lambda e: e.activation(out=eg, in_=eg, func=AF.Exp), reads=R, writes=R)
        dv(lambda e: e.tensor_reduce(out=gp, in_=eg, axis=AX.X, op=ALU.add))
        dv(lambda e: e.reciprocal(out=gp, in_=gp))
        dv(lambda e: e.tensor_scalar(out=eg, in0=G, scalar1=BIG, scalar2=-BIG, op0=ALU.mult, op1=ALU.add))
        Le4 = Le.rearrange("p t (g x) -> p t g x", g=4)
        Lp4 = Lp.rearrange("p t (g x) -> p t g x", g=4)
        dv(lambda e: e.tensor_tensor(out=Lp4, in0=Le4, in1=eg.unsqueeze(3).to_broadcast([128, NT, 4, 4]), op=ALU.add), extra_r=["logits"])
        dv(lambda e: e.tensor_reduce(out=m1, in_=Lp, axis=AX.X, op=ALU.max))
        dv(lambda e: e.tensor_tensor(out=eq1, in0=Lp, in1=bc3(m1, 16), op=ALU.is_equal))
        dv(lambda e: e.scalar_tensor_tensor(out=L2, in0=eq1, scalar=-BIG, in1=Lp, op0=ALU.mult, op1=ALU.add))
        dv(lambda e: e.tensor_reduce(out=m2, in_=L2, axis=AX.X, op=ALU.max))
        dv(lambda e: e.tensor_tensor(out=Ssel, in0=Lp, in1=bc3(m2, 16), op=ALU.is_ge))
        dv(lambda e: e.tensor_tensor(out=ew, in0=Lp, in1=bc3(m1, 16), op=ALU.subtract))
        S.op("act", lambda e: e.activation(out=ew, in_=ew, func=AF.Exp), reads=R, writes=R)
        dv(lambda e: e.tensor_tensor(out=m2, in0=m2, in1=m1, op=ALU.subtract))
        S.op("act", lambda e: e.activation(out=m2, in_=m2, func=AF.Exp), reads=R, writes=R)
        dv(lambda e: e.tensor_scalar(out=m2, in0=m2, scalar1=1.0, scalar2=None, op0=ALU.add))
        dv(lambda e: e.reciprocal(out=m2, in_=m2))
        dv(lambda e: e.tensor_tensor(out=gp, in0=gp, in1=m2, op=ALU.mult))
        dv(lambda e: e.tensor_tensor(out=Wt, in0=ew, in1=Ssel, op=ALU.mult))
        dv(lambda e: e.tensor_tensor(out=Wt, in0=Wt, in1=bc3(gp, 16), op=ALU.mult))
        dv(lambda e: e.tensor_copy(S_bf, Ssel))
        Sflat = S_bf.rearrange("p t x -> p (t x)")
        for hf in range(2):
            S.op("pe", (lambda hf: lambda e: e.matmul(ps[:, hf, :], lhsT=ones_bf, rhs=Sflat[:, hf * 512:(hf + 1) * 512], start=True, stop=True))(hf), reads=R + ["ones_bf"], writes=[PB(hf)])
            S.op("dve", (lambda hf: lambda e: e.tensor_copy(totS.rearrange("p t x -> p (t x)")[:, hf * 512:(hf + 1) * 512], ps[:, hf, :]))(hf), reads=R + [PB(hf)], writes=R + [PB(hf)])
        for T in range(NT):
            bk = 2 + T // 32
            off = (T % 32) * 16
            S.op("pe", (lambda T, bk, off: lambda e: e.matmul(ps[:, bk, off:off + 16], lhsT=U_bf, rhs=S_bf[:, T, :], start=(T % 32 == 0), stop=(T % 32 == 31), skip_group_check=True))(T, bk, off), reads=R + ["U_bf"], writes=[PB(bk)])
        for hf in range(2):
            S.op("dve", (lambda hf: lambda e: e.tensor_copy(rankS.rearrange("p t x -> p (t x)")[:, hf * 512:(hf + 1) * 512], ps[:, 2 + hf, :]))(hf), reads=R + [PB(2 + hf)], writes=R + [PB(2 + hf)])
        dv(lambda e: e.tensor_copy(L2, totS))
        src, dst = totS, totB
        sh = 1
        while sh < NT:
            dv((lambda src, dst, sh: lambda e: e.tensor_copy(dst[:, 0:sh, :], src[:, 0:sh, :]))(src, dst, sh))
            dv((lambda src, dst, sh: lambda e: e.tensor_tensor(out=dst[:, sh:NT, :], in0=src[:, sh:NT, :], in1=src[:, 0:NT - sh, :], op=ALU.add))(src, dst, sh))
            src, dst = dst, src
            sh *= 2
        incl = src
        dv(lambda e: e.tensor_copy(cntv, incl[:, NT - 1, :]))
        dv(lambda e: e.tensor_tensor(out=incl, in0=incl, in1=L2, op=ALU.subtract))
        S.op("pool", lambda e: e.iota(thr_i, [[0, 16], [512, 32]], base=0, channel_multiplier=0), reads=R, writes=R)
        dv(lambda e: e.tensor_copy(thr, thr_i))
        dv(lambda e: e.tensor_tensor(out=cmp, in0=cntv.unsqueeze(2).to_broadcast([128, 16, 32]), in1=thr, op=ALU.is_gt))
        dv(lambda e: e.tensor_reduce(out=padded, in_=cmp, axis=AX.X, op=ALU.add))
        dv(lambda e: e.tensor_scalar(out=padded, in0=padded, scalar1=512.0, scalar2=None, op0=ALU.mult))
        dv(lambda e: e.memset(pst[:, 0:1], 0.0))
        for ei in range(1, 16):
            dv((lambda ei: lambda e: e.tensor_tensor(out=pst[:, ei:ei + 1], in0=pst[:, ei - 1:ei], in1=padded[:, ei - 1:ei], op=ALU.add))(ei))
        dv(lambda e: e.tensor_tensor(out=pend, in0=pst, in1=padded, op=ALU.add))
        dv(lambda e: e.tensor_tensor(out=incl, in0=incl, in1=rankS, op=ALU.add))
        dv(lambda e: e.tensor_tensor(out=incl, in0=incl, in1=pst.unsqueeze(1).to_broadcast([128, NT, 16]), op=ALU.add))
        dv(lambda e: e.scalar_tensor_tensor(out=incl, in0=incl, scalar=1.0, in1=Ssel, op0=ALU.add, op1=ALU.mult))
        dv(lambda e: e.tensor_scalar(out=incl, in0=incl, scalar1=-1.0, scalar2=None, op0=ALU.add))
        dv(lambda e: e.tensor_reduce(out=dhi, in_=incl, axis=AX.X, op=ALU.max))
        dv(lambda e: e.tensor_tensor(out=eq1, in0=incl, in1=bc3(dhi, 16), op=ALU.is_equal))
        dv(lambda e: e.scalar_tensor_tensor(out=L2, in0=eq1, scalar=-1.0e6, in1=incl, op0=ALU.mult, op1=ALU.add))
        dv(lambda e: e.tensor_reduce(out=dlo, in_=L2, axis=AX.X, op=ALU.max))
        dv(lambda e: e.tensor_tensor(out=L2, in0=eq1, in1=Wt, op=ALU.mult))
        dv(lambda e: e.tensor_reduce(out=whi, in_=L2, axis=AX.X, op=ALU.add), extra_w=["whi"])
        dv(lambda e: e.tensor_tensor(out=L2, in0=Ssel, in1=eq1, op=ALU.subtract))
        dv(lambda e: e.tensor_tensor(out=L2, in0=L2, in1=Wt, op=ALU.mult))
        dv(lambda e: e.tensor_reduce(out=wlo, in_=L2, axis=AX.X, op=ALU.add), extra_w=["wlo"])
        dv(lambda e: e.tensor_copy(dhi_i, dhi), extra_w=["dhi_i"])
        dv(lambda e: e.tensor_copy(dlo_i, dlo), extra_w=["dlo_i"])
        S.op("pool", lambda e: e.iota(bst_i, [[512, NBLK]], base=0, channel_multiplier=0), reads=R, writes=R)
        S.op("pool", lambda e: e.iota(pk_i, [[128, 8]], base=0, channel_multiplier=1), reads=R, writes=R)
        dv(lambda e: e.tensor_copy(bst, bst_i))
        dv(lambda e: e.tensor_copy(pk, pk_i))
        dv(lambda e: e.tensor_tensor(out=cmpb, in0=pend.unsqueeze(1).to_broadcast([128, NBLK, 16]), in1=bst.unsqueeze(2).to_broadcast([128, NBLK, 16]), op=ALU.is_le))
        dv(lambda e: e.tensor_reduce(out=be, in_=cmpb, axis=AX.X, op=ALU.add))
        dv(lambda e: e.tensor_scalar(out=be, in0=be, scalar1=15.0, scalar2=None, op0=ALU.min))
        dv(lambda e: e.scalar_tensor_tensor(out=idxf, in0=be, scalar=128.0, in1=pk[:, 0:1].to_broadcast([128, NBLK]), op0=ALU.mult, op1=ALU.add))
        dv(lambda e: e.tensor_copy(idxe_i, idxf), extra_w=["idxe_i"])
        sbuf2 = [A.alloc([128, D], BF16) for _ in range(4)]
        for T in range(NT):
            sbb = sbuf2[T % 4]
            sk = ("sbuf2", T % 4)
            S.dma("sp", (lambda T, sbb: lambda e: e.dma_start(out=sbb, in_=h2s[T * 128:(T + 1) * 128, :]))(T, sbb), sk, writes=[sk])
            for di, dk_ in ((dhi_i, "dhi_i"), (dlo_i, "dlo_i")):
                S.dma("pool", (lambda T, sbb, di: lambda e: e.indirect_dma_start(out=rows, out_offset=bass.IndirectOffsetOnAxis(ap=di[:, T:T + 1], axis=0), in_=sbb, in_offset=None))(T, sbb, di), ("scat", T % 4), reads=[sk, dk_])
        S.barrier()

        p3_off = A.off
        wg_e = [A.alloc([128, 8, 512], BF16) for _ in range(2)]
        wu_e = [A.alloc([128, 8, 512], BF16) for _ in range(2)]
        wd_e = [A.alloc([128, 4, D], BF16) for _ in range(2)]
        xr = [A.alloc([128, 4, D], BF16) for _ in range(3)]
        XTb = [A.alloc([128, 8, 512], BF16) for _ in range(2)]
        th = [A.alloc([128, 512], F32) for _ in range(2)]
        ga = [A.alloc([128, 512], F32) for _ in range(2)]
        hidT = A.alloc([128, 4, 512], BF16)
        yb = [A.alloc([128, D], BF16) for _ in range(2)]
        allb = tuple(range(8))
        def xr_load(bq):
            iq = bq % 3
            S.dma("sp", lambda e: e.dma_start(out=xr[iq], in_=rows[bq * 512:(bq + 1) * 512, :].rearrange("(t p) d -> p t d", p=128)), ("xr", iq), writes=[("xr", iq)])

        def xt_transposes(bq):
            iq = bq % 2
            ir = bq % 3
            for t in range(4):
                transpose8(xr[ir][:, t, :], ("xr", ir), XTb[iq][:, :, t * 128:(t + 1) * 128], ("XT", iq), next_bank(allb), evac_eng())

        xr_load(0)
        xr_load(1)
        for blk in range(NBLK):
            i = blk % 2
            for mi, (wb, wkey) in enumerate(((wg_e, "wg_e"), (wu_e, "wu_e"), (wd_e, "wd_e"))):
                S.dma("pool", (lambda mi, wb, i, blk: lambda e: e.indirect_dma_start(out=wb[i].rearrange("p k n -> p (k n)"), out_offset=None, in_=wexp[mi], in_offset=bass.IndirectOffsetOnAxis(ap=idxe_i[:, blk:blk + 1], axis=0)))(mi, wb, i, blk), (wkey, i), writes=[(wkey, i)])
            if blk + 2 < NBLK:
                xr_load(blk + 2)
            XT = XTb[i]
            xtk = ("XT", i)
            if blk == 0:
                xt_transposes(0)
            for fc in range(4):
                bg = next_bank(allb)
                bu = next_bank(allb)
                for kc in range(8):
                    S.op("pe", (lambda fc, kc, bg, i: lambda e: e.matmul(ps[:, bg, :], lhsT=wg_e[i][:, kc, fc * 128:(fc + 1) * 128], rhs=XTb[i][:, kc, :], start=(kc == 0), stop=(kc == 7)))(fc, kc, bg, i), reads=[("wg_e", i), xtk], writes=[PB(bg)])
                for kc in range(8):
                    S.op("pe", (lambda fc, kc, bu, i: lambda e: e.matmul(ps[:, bu, :], lhsT=wu_e[i][:, kc, fc * 128:(fc + 1) * 128], rhs=XTb[i][:, kc, :], start=(kc == 0), stop=(kc == 7)))(fc, kc, bu, i), reads=[("wu_e", i), xtk], writes=[PB(bu)])
                f2 = fc % 2
                S.op("act", (lambda bg, f2: lambda e: e.activation(out=th[f2], in_=ps[:, bg, :], func=AF.Tanh, scale=0.5))(bg, f2), reads=[PB(bg)], writes=[("th", f2), PB(bg)])
                S.op("dve", (lambda bg, f2: lambda e: e.scalar_tensor_tensor(out=ga[f2], in0=th[f2], scalar=1.0, in1=ps[:, bg, :], op0=ALU.add, op1=ALU.mult))(bg, f2), reads=[("th", f2), PB(bg)], writes=[("ga", f2), PB(bg)])
                S.op("dve", (lambda bu, f2, fc: lambda e: e.scalar_tensor_tensor(out=hidT[:, fc, :], in0=ga[f2], scalar=0.5, in1=ps[:, bu, :], op0=ALU.mult, op1=ALU.mult))(bu, f2, fc), reads=[("ga", f2), PB(bu)], writes=["hidT", PB(bu)])
            if blk + 1 < NBLK:
                xt_transposes(blk + 1)
            for t in range(4):
                ybb = yb[t % 2]
                yk = ("yb", t % 2)
                for hf in range(2):
                    bk = next_bank(allb)
                    for fc in range(4):
                        S.op("pe", (lambda t, hf, fc, bk, i: lambda e: e.matmul(ps[:, bk, :], lhsT=hidT[:, fc, t * 128:(t + 1) * 128], rhs=wd_e[i][:, fc, hf * 512:(hf + 1) * 512], start=(fc == 0), stop=(fc == 3)))(t, hf, fc, bk, i), reads=["hidT", ("wd_e", i)], writes=[PB(bk)])
                    copy_op("act" if hf == 0 else "dve", ybb[:, hf * 512:(hf + 1) * 512], ps[:, bk, :], [PB(bk)], [yk, PB(bk)])
                r0 = blk * 512 + t * 128
                S.dma("sp", (lambda r0, ybb: lambda e: e.dma_start(out=yrows[r0:r0 + 128, :], in_=ybb))(r0, ybb), yk, reads=[yk])
        S.barrier()

        A.off = p3_off
        NB4 = 4
        x1b = [A.alloc([128, D], F32) for _ in range(NB4)]
        ya = [A.alloc([128, D], BF16) for _ in range(NB4)]
        yc = [A.alloc([128, D], BF16) for _ in range(NB4)]
        ob = [A.alloc([128, D], F32) for _ in range(NB4)]
        junk2 = A.alloc([128, D], BF16)
        gfin_b = A.alloc([128, D], F32)
        S.dma("sp", lambda e: e.dma_start(out=gfin_b, in_=gfin.partition_broadcast(128)), "gfin_b", writes=["gfin_b"])
        ssf = A.alloc([128, NT], F32)
        def p4_load(T):
            i = T % NB4
            S.dma("sp", lambda e: e.dma_start(out=x1b[i], in_=x1s[T * 128:(T + 1) * 128, :]), ("x1b", i), writes=[("x1b", i)])
            S.dma("pool", lambda e: e.indirect_dma_start(out=ya[i], out_offset=None, in_=yrows, in_offset=bass.IndirectOffsetOnAxis(ap=dhi_i[:, T:T + 1], axis=0)), ("ya", i), writes=[("ya", i)])
            S.dma("pool", lambda e: e.indirect_dma_start(out=yc[i], out_offset=None, in_=yrows, in_offset=bass.IndirectOffsetOnAxis(ap=dlo_i[:, T:T + 1], axis=0)), ("yc", i), writes=[("yc", i)])

        def p4_a(T):
            i = T % NB4
            S.op("dve", lambda e: e.scalar_tensor_tensor(out=x1b[i], in0=ya[i], scalar=whi[:, T:T + 1], in1=x1b[i], op0=ALU.mult, op1=ALU.add), reads=[("ya", i), ("x1b", i)], writes=[("x1b", i)])
            S.op("dve", lambda e: e.scalar_tensor_tensor(out=x1b[i], in0=yc[i], scalar=wlo[:, T:T + 1], in1=x1b[i], op0=ALU.mult, op1=ALU.add), reads=[("yc", i), ("x1b", i)], writes=[("x1b", i)])
            S.op("act", lambda e: e.activation(out=junk2, in_=x1b[i], func=AF.Square, accum_out=ssf[:, T:T + 1]), reads=[("x1b", i)], writes=["junk2", ("ssf", T)])

        def p4_b(T):
            i = T % NB4
            S.op("dve", lambda e: e.tensor_scalar(out=ssf[:, T:T + 1], in0=ssf[:, T:T + 1], scalar1=1.0 / D, scalar2=EPS, op0=ALU.mult, op1=ALU.add), reads=[("ssf", T)], writes=[("ssf", T)])
            S.op("act", lambda e: e.activation(out=ssf[:, T:T + 1], in_=ssf[:, T:T + 1], func=AF.Sqrt), reads=[("ssf", T)], writes=[("ssf", T)])

        def p4_c(T):
            i = T % NB4
            S.op("dve", lambda e: e.reciprocal(out=ssf[:, T:T + 1], in_=ssf[:, T:T + 1]), reads=[("ssf", T)], writes=[("ssf", T)])
            S.op("dve", lambda e: e.scalar_tensor_tensor(out=ob[i], in0=x1b[i], scalar=ssf[:, T:T + 1], in1=gfin_b, op0=ALU.mult, op1=ALU.mult), reads=[("x1b", i), ("ssf", T), "gfin_b"], writes=[("ob", i)])
            S.dma("sp", lambda e: e.dma_start(out=out[T * 128:(T + 1) * 128, :], in_=ob[i]), ("ob", i), reads=[("ob", i)])

        NB4 = 6
        x1b = x1b + [A.alloc([128, D], F32) for _ in range(2)]
        ya = ya + [A.alloc([128, D], BF16) for _ in range(2)]
        yc = yc + [A.alloc([128, D], BF16) for _ in range(2)]
        ob = ob + [A.alloc([128, D], F32) for _ in range(2)]
        for T in range(NB4 - 3):
            p4_load(T)
        for T in range(NT + 2):
            if T + NB4 - 3 < NT:
                p4_load(T + NB4 - 3)
            if T < NT:
                p4_a(T)
            if 0 <= T - 1 < NT:
                p4_b(T - 1)
            if 0 <= T - 2 < NT:
                p4_c(T - 2)
        S.emit()
    return nc


_NC_CACHE = {}


def _prep_inputs(inp):
    f = lambda a: np.ascontiguousarray(np.asarray(a, dtype=np.float32))
    w_in = f(inp["w_in"])[0]
    gates = w_in[:, 2048:].reshape(8, 128, 3, 8, 128).transpose(3, 1, 0, 2, 4).reshape(8, 128, 24, 128)
    pp = f(inp["p_pool"])[0].reshape(2, 128, 8, 128).transpose(2, 1, 0, 3)
    pd = f(inp["p_diff"])[0].reshape(4, 128, 8, 128).transpose(2, 1, 0, 3)
    pm = f(inp["p_mem"])[0].reshape(2, 128, 8, 128).transpose(2, 1, 0, 3)
    w_fc = np.ascontiguousarray(np.concatenate([gates, pp, pd, pm], axis=2)).reshape(8, 128, 32 * 128)
    shared = {
        "gmix": np.ascontiguousarray(f(inp["norm_mix_g"]).reshape(8, 128).T),
        "gmem": f(inp["norm_mem_g"]).reshape(1, D),
        "gffn": f(inp["norm_ffn_g"]).reshape(1, D),
        "gfin": f(inp["final_g"]).reshape(1, D),
        "w_in_a": np.ascontiguousarray(w_in[:, :2048]),
        "w_fc": w_fc,
        "pool_w": f(inp["pool_w"])[0],
        "pscale": np.ascontiguousarray(f(inp["pool_scale"])[0].reshape(2, 128).T),
        "lamv": np.concatenate([f(inp["lam_q1"])[0], f(inp["lam_k1"])[0], f(inp["lam_q2"])[0], f(inp["lam_k2"])[0]]).reshape(1, 256),
        "subg": f(inp["subln_g"]).reshape(1, 128),
        "wkv": f(inp["w_mem_kv"])[0],
        "w_o": f(inp["w_o"])[0],
        "w_r": np.ascontiguousarray(np.concatenate([f(inp["w_router_group"])[0], f(inp["w_router_expert"])[0].reshape(D, 16)], axis=1)),
        "b_r": np.concatenate([f(inp["b_router_group"])[0], f(inp["b_router_expert"])[0].reshape(16)]).reshape(1, 20),
        "w_gate": f(inp["w_expert_gate"])[0].reshape(16 * 1024, 512),
        "w_up": f(inp["w_expert_up"])[0].reshape(16 * 1024, 512),
        "w_down": f(inp["w_expert_down"])[0].reshape(16 * 512, 1024),
    }
    x = f(inp["x"])
    mem = f(inp["mem"])
    maps = []
    for c in range(8):
        m = dict(shared)
        m["x"] = x[4 * c:4 * c + 4].reshape(NTOK, D)
        m["mem"] = mem[4 * c:4 * c + 4].reshape(1024, D)
        maps.append(m)
    return maps


def kernel(**inputs):
    stage = int(inputs.pop("_stage", 9)) if "_stage" in inputs else 9
    if stage not in _NC_CACHE:
        _NC_CACHE[stage] = build_nc(stage)
    nc = _NC_CACHE[stage]
    maps = _prep_inputs(inputs)
    res = run_bass_kernel_spmd(nc, maps, core_ids=list(range(8)))
    outs = [np.asarray(r["out"], dtype=np.float32).reshape(4, 2048, D) for r in res.results]
    return np.concatenate(outs, axis=0)
```
